# Optimizing a Trainium2 kernel written in Bass

```python
import jax
import jax.numpy as jnp
from jax import lax
import numpy as np

D_MODEL = 1024
BATCH = 32
SEQ = 2048
DEPTH = 2

GRID_W = 64
CTX_LEN = 256
D_MIX = D_MODEL
EPS = 1e-6
HEAD_DIM = 64
ATT_HEADS = 8
ATT_KV_HEADS = 2
ATT_GROUP = ATT_HEADS // ATT_KV_HEADS
ATT_WIDTH = ATT_HEADS * HEAD_DIM
WINDOW = 128
ATT_BLOCK = 128
ROPE_BASE = 10000.0
GLA_HEADS = 4
GLA_DV = 64
GLA_DK = 32
GLA_WIDTH = GLA_HEADS * GLA_DV
GLA_GATE_RANK = 16
GLA_GATE_NORM = 16.0
GLA_CHUNK = 64
CONV_WIDTH = D_MIX - ATT_WIDTH - GLA_WIDTH
CONV_K = 3
N_EXPERTS = 16
N_GROUPS = 4
EXPERTS_PER_GROUP = N_EXPERTS // N_GROUPS
TOP_K = 2
D_EXPERT = D_MODEL // 2
IN_SIZES = (ATT_WIDTH, ATT_KV_HEADS * HEAD_DIM, ATT_KV_HEADS * HEAD_DIM,
            GLA_HEADS * GLA_DK, GLA_HEADS * GLA_DK, GLA_WIDTH, GLA_WIDTH, GLA_GATE_RANK, GLA_GATE_RANK,
            CONV_WIDTH, CONV_WIDTH, CONV_WIDTH)
N_IN = sum(IN_SIZES)

kernel_name = 'hybrid_dit_swa_gla_shortconv_grouped_moe'


def _rms_norm(x, g):
    xf = x.astype(jnp.float32)
    y = xf * lax.rsqrt(jnp.mean(xf * xf, axis=-1, keepdims=True) + EPS)
    return (y * g.astype(jnp.float32)).astype(x.dtype)


def _modulation(cond, w, b):
    m = (jax.nn.silu(cond) @ w + b)[..., None, :]
    return jnp.split(m, 6, axis=-1)


def _modulate(h, shift, scale):
    return h * (1.0 + scale) + shift


def _split_columns(p):
    parts, start = [], 0
    for size in IN_SIZES:
        parts.append(p[..., start:start + size])
        start += size
    return parts


def _axial_rope_tables(n_tokens):
    rows = n_tokens // GRID_W
    row, col = jnp.meshgrid(jnp.arange(rows), jnp.arange(GRID_W), indexing='ij')
    n_freq = HEAD_DIM // 4
    inv_freq = ROPE_BASE ** (-jnp.arange(n_freq, dtype=jnp.float32) / n_freq)
    ang = jnp.concatenate([row.reshape(-1, 1).astype(jnp.float32) * inv_freq,
                           col.reshape(-1, 1).astype(jnp.float32) * inv_freq], axis=-1)
    return jnp.cos(ang), jnp.sin(ang)


def _rope(x, cos, sin):
    x1, x2 = jnp.split(x, 2, axis=-1)
    cos = cos[None, :, None, :].astype(x.dtype)
    sin = sin[None, :, None, :].astype(x.dtype)
    return jnp.concatenate([x1 * cos - x2 * sin, x1 * sin + x2 * cos], axis=-1)


def _softmax_with_sink(scores, sink):
    sink_col = jnp.broadcast_to(sink[None, :, :, None, None], scores.shape[:-1] + (1,))
    return jax.nn.softmax(jnp.concatenate([scores, sink_col], axis=-1), axis=-1)[..., :-1]


def _attention_group(q_l, k_l, v_l, q_c, k_c, v_c, q_norm_g, k_norm_g, sink, cos, sin, with_ctx_out):
    B, S, _ = q_l.shape
    L = q_c.shape[1]
    scale = HEAD_DIM ** -0.5
    sink = sink.reshape(ATT_KV_HEADS, ATT_GROUP).astype(jnp.float32)
    q_l = (_rope(_rms_norm(q_l.reshape(B, S, ATT_HEADS, HEAD_DIM), q_norm_g), cos, sin) * scale
           ).reshape(B, S, ATT_KV_HEADS, ATT_GROUP, HEAD_DIM)
    k_l = _rope(_rms_norm(k_l.reshape(B, S, ATT_KV_HEADS, HEAD_DIM), k_norm_g), cos, sin)
    v_l = v_l.reshape(B, S, ATT_KV_HEADS, HEAD_DIM)
    k_c = _rms_norm(k_c.reshape(B, L, ATT_KV_HEADS, HEAD_DIM), k_norm_g)
    v_c = v_c.reshape(B, L, ATT_KV_HEADS, HEAD_DIM)

    span = ATT_BLOCK + 2 * WINDOW
    pad = ((0, 0), (WINDOW, WINDOW), (0, 0), (0, 0))
    k_pad, v_pad = jnp.pad(k_l, pad), jnp.pad(v_l, pad)

    def block(i):
        start = i * ATT_BLOCK
        qb = lax.dynamic_slice_in_dim(q_l, start, ATT_BLOCK, axis=1)
        kb = lax.dynamic_slice_in_dim(k_pad, start, span, axis=1)
        vb = lax.dynamic_slice_in_dim(v_pad, start, span, axis=1)
        qpos = start + jnp.arange(ATT_BLOCK)
        kpos = start - WINDOW + jnp.arange(span)
        valid = (jnp.abs(qpos[:, None] - kpos[None, :]) <= WINDOW) & (kpos >= 0)[None, :] & (kpos < S)[None, :]
        s_win = jnp.einsum('bqhgd,bshd->bhgqs', qb, kb).astype(jnp.float32)
        s_win = jnp.where(valid, s_win, -jnp.inf)
        s_ctx = jnp.einsum('bqhgd,bchd->bhgqc', qb, k_c).astype(jnp.float32)
        p = _softmax_with_sink(jnp.concatenate([s_win, s_ctx], axis=-1), sink).astype(v_l.dtype)
        return (jnp.einsum('bhgqs,bshd->bqhgd', p[..., :span], vb)
                + jnp.einsum('bhgqc,bchd->bqhgd', p[..., span:], v_c))

    o = lax.map(block, jnp.arange(S // ATT_BLOCK))
    y_l = jnp.moveaxis(o, 0, 1).reshape(B, S, ATT_WIDTH)
    if not with_ctx_out:
        return y_l, None
    q_c = (_rms_norm(q_c.reshape(B, L, ATT_HEADS, HEAD_DIM), q_norm_g) * scale
           ).reshape(B, L, ATT_KV_HEADS, ATT_GROUP, HEAD_DIM)
    s = jnp.einsum('bqhgd,bchd->bhgqc', q_c, k_c).astype(jnp.float32)
    p = _softmax_with_sink(s, sink).astype(v_c.dtype)
    y_c = jnp.einsum('bhgqc,bchd->bqhgd', p, v_c).reshape(B, L, ATT_WIDTH)
    return y_l, y_c


def _gla_chunk_scan(q, k, v, g, s0, include_diag):
    B, H, T, _ = q.shape
    n = T // GLA_CHUNK

    def to_chunks(a):
        return a.reshape(B, H, n, GLA_CHUNK, a.shape[-1]).transpose(2, 0, 1, 3, 4)

    idx = jnp.arange(GLA_CHUNK)
    mask = (idx[:, None] >= idx[None, :]) if include_diag else (idx[:, None] > idx[None, :])

    def step(state, xs):
        qi, ki, vi, gi = xs
        b = jnp.cumsum(gi, axis=-2)
        o_inter = jnp.einsum('bhcd,bhde->bhce', qi * jnp.exp(b), state)
        diff = b[..., :, None, :] - b[..., None, :, :]
        decay = jnp.exp(jnp.where(mask[:, :, None], diff, -jnp.inf))
        att = jnp.einsum('bhid,bhjd,bhijd->bhij', qi, ki, decay)
        o_intra = jnp.einsum('bhij,bhje->bhie', att, vi)
        b_last = b[..., -1:, :]
        new_state = (jnp.exp(b_last[..., 0, :])[..., None] * state
                     + jnp.einsum('bhcd,bhce->bhde', ki * jnp.exp(b_last - b), vi))
        return new_state, o_inter + o_intra

    s_fin, o = lax.scan(step, s0, (to_chunks(q), to_chunks(k), to_chunks(v), to_chunks(g)))
    return o.transpose(1, 2, 0, 3, 4).reshape(B, H, T, v.shape[-1]), s_fin


def _bidir_gla(q, k, v, g_fwd, g_bwd, s_fwd0, s_bwd0):
    o_f, s_f = _gla_chunk_scan(q, k, v, g_fwd, s_fwd0, True)
    flip = lambda a: jnp.flip(a, axis=2)
    o_b, s_b = _gla_chunk_scan(flip(q), flip(k), flip(v), flip(g_bwd), s_bwd0, False)
    return o_f + flip(o_b), s_f, s_b


def _gla_heads(q, k, v, g_f, g_b, gate_w, gate_b):
    B, T, _ = q.shape

    def heads(a, d):
        return a.reshape(B, T, GLA_HEADS, d).transpose(0, 2, 1, 3)

    def log_decay(g_low, d):
        z = (g_low @ gate_w[d] + gate_b[d]).astype(jnp.float32)
        return heads(jax.nn.log_sigmoid(z) / GLA_GATE_NORM, GLA_DK)

    return (heads(q, GLA_DK) * GLA_DK ** -0.5, heads(k, GLA_DK), heads(v, GLA_DV),
            log_decay(g_f, 0), log_decay(g_b, 1))


def _gla_out(o, r, norm_g):
    B, H, T, DV = o.shape
    o = _rms_norm(o, norm_g).transpose(0, 2, 1, 3).reshape(B, T, H * DV)
    return o.astype(r.dtype) * jax.nn.silu(r)


def _gla_group(parts_l, parts_c, gate_w, gate_b, norm_g, with_ctx_out):
    q_l, k_l, v_l, r_l, gf_l, gb_l = parts_l
    q_c, k_c, v_c, r_c, gf_c, gb_c = parts_c
    ql, kl, vl, dfl, dbl = _gla_heads(q_l, k_l, v_l, gf_l, gb_l, gate_w, gate_b)
    qc, kc, vc, dfc, dbc = _gla_heads(q_c, k_c, v_c, gf_c, gb_c, gate_w, gate_b)
    zeros = jnp.zeros((q_l.shape[0], GLA_HEADS, GLA_DK, GLA_DV), jnp.float32)
    o_c, s_f, s_b = _bidir_gla(qc, kc, vc, dfc, dbc, zeros, zeros)
    o_l, _, _ = _bidir_gla(ql, kl, vl, dfl, dbl, s_f, s_b)
    y_l = _gla_out(o_l, r_l, norm_g)
    y_c = _gla_out(o_c, r_c, norm_g) if with_ctx_out else None
    return y_l, y_c


def _short_conv(u, w):
    kernel = w[:, None, :].astype(u.dtype)
    return lax.conv_general_dilated(u, kernel, window_strides=(1,), padding=((CONV_K // 2, CONV_K // 2),),
                                    dimension_numbers=('NWC', 'WIO', 'NWC'), feature_group_count=u.shape[-1])


def _token_mixers(p_lat, p_ctx, cos, sin, q_norm_g, k_norm_g, sink, gla_gate_w, gla_gate_b, gla_norm_g,
                  conv_w, with_ctx_out):
    aq_l, ak_l, av_l, gq_l, gk_l, gv_l, gr_l, gf_l, gb_l, cb_l, cc_l, ch_l = _split_columns(p_lat)
    aq_c, ak_c, av_c, gq_c, gk_c, gv_c, gr_c, gf_c, gb_c, cb_c, cc_c, ch_c = _split_columns(p_ctx)
    att_l, att_c = _attention_group(aq_l, ak_l, av_l, aq_c, ak_c, av_c, q_norm_g, k_norm_g, sink,
                                    cos, sin, with_ctx_out)
    gla_l, gla_c = _gla_group((gq_l, gk_l, gv_l, gr_l, gf_l, gb_l), (gq_c, gk_c, gv_c, gr_c, gf_c, gb_c),
                              gla_gate_w, gla_gate_b, gla_norm_g, with_ctx_out)
    conv_l = cb_l * _short_conv(cc_l * ch_l, conv_w)
    y_lat = jnp.concatenate([att_l, gla_l, conv_l], axis=-1)
    if not with_ctx_out:
        return y_lat, None
    conv_c = cb_c * _short_conv(cc_c * ch_c, conv_w)
    y_ctx = jnp.concatenate([att_c, gla_c, conv_c], axis=-1)
    return y_lat, y_ctx


def _moe(h, w_router, b_router, w_gate, w_up, w_down):
    logits = jnp.einsum('btd,de->bte', h, w_router).astype(jnp.float32) + b_router.astype(jnp.float32)
    probs = jax.nn.softmax(logits, axis=-1)
    grouped = probs.reshape(probs.shape[:-1] + (N_GROUPS, EXPERTS_PER_GROUP))
    group_score = lax.top_k(grouped, TOP_K)[0].sum(axis=-1)
    g_sel = jnp.argmax(group_score, axis=-1)
    in_group = jnp.take_along_axis(grouped, g_sel[..., None, None], axis=-2)[..., 0, :]
    top_p, top_i = lax.top_k(in_group, TOP_K)
    weights = top_p / jnp.sum(top_p, axis=-1, keepdims=True)
    expert_idx = g_sel[..., None] * EXPERTS_PER_GROUP + top_i
    gates = jnp.sum(jax.nn.one_hot(expert_idx, N_EXPERTS, dtype=jnp.float32) * weights[..., None],
                    axis=-2).astype(h.dtype)
    out = jnp.zeros_like(h)
    for e in range(N_EXPERTS):
        a = jax.nn.silu(h @ w_gate[e]) * (h @ w_up[e])
        out = out + gates[..., e:e + 1] * (a @ w_down[e])
    return out


def setup_inputs(seed: int = 0) -> dict:
    key = jax.random.key(seed)
    ks = jax.random.split(key, 22)

    def nrm(k, shape, scale):
        return jax.random.normal(k, shape, jnp.float32) * scale

    return {
        'x': nrm(ks[0], (BATCH, SEQ, D_MODEL), 1.0),
        'c': nrm(ks[1], (BATCH, D_MODEL), 1.0),
        'ctx': nrm(ks[2], (BATCH, CTX_LEN, D_MODEL), 1.0),
        'c_ctx': nrm(ks[3], (D_MODEL,), 1.0),
        'w_ada': nrm(ks[4], (DEPTH, D_MODEL, 6 * D_MODEL), 0.3 * D_MODEL ** -0.5),
        'b_ada': nrm(ks[5], (DEPTH, 6 * D_MODEL), 0.05),
        'norm_mix_g': 1.0 + nrm(ks[6], (DEPTH, D_MODEL), 0.05),
        'norm_ffn_g': 1.0 + nrm(ks[7], (DEPTH, D_MODEL), 0.05),
        'w_in': nrm(ks[8], (DEPTH, D_MODEL, N_IN), D_MODEL ** -0.5),
        'q_norm_g': 1.0 + nrm(ks[9], (DEPTH, HEAD_DIM), 0.05),
        'k_norm_g': 1.0 + nrm(ks[10], (DEPTH, HEAD_DIM), 0.05),
        'attn_sink': nrm(ks[11], (DEPTH, ATT_HEADS), 0.5),
        'gla_gate_w': nrm(ks[12], (DEPTH, 2, GLA_GATE_RANK, GLA_HEADS * GLA_DK), GLA_GATE_RANK ** -0.5),
        'gla_gate_b': nrm(ks[13], (DEPTH, 2, GLA_HEADS * GLA_DK), 0.1),
        'gla_norm_g': 1.0 + nrm(ks[14], (DEPTH, GLA_DV), 0.05),
        'conv_w': nrm(ks[15], (DEPTH, CONV_K, CONV_WIDTH), CONV_K ** -0.5),
        'w_out': nrm(ks[16], (DEPTH, D_MIX, D_MODEL), D_MIX ** -0.5),
        'w_router': nrm(ks[17], (D_MODEL, N_EXPERTS), D_MODEL ** -0.5),
        'b_router': nrm(ks[18], (N_EXPERTS,), 0.01),
        'w_gate_e': nrm(ks[19], (DEPTH, N_EXPERTS, D_MODEL, D_EXPERT), D_MODEL ** -0.5),
        'w_up_e': nrm(ks[20], (DEPTH, N_EXPERTS, D_MODEL, D_EXPERT), D_MODEL ** -0.5),
        'w_down_e': nrm(ks[21], (DEPTH, N_EXPERTS, D_EXPERT, D_MODEL), D_EXPERT ** -0.5),
    }


def reference(x, c, ctx, c_ctx, w_ada, b_ada, norm_mix_g, norm_ffn_g, w_in, q_norm_g, k_norm_g, attn_sink,
              gla_gate_w, gla_gate_b, gla_norm_g, conv_w, w_out, w_router, b_router, w_gate_e, w_up_e, w_down_e):
    cos, sin = _axial_rope_tables(x.shape[1])
    ctx_len = ctx.shape[1]
    for l in range(DEPTH):
        last = l == DEPTH - 1
        m_lat = _modulation(c, w_ada[l], b_ada[l])
        m_ctx = _modulation(c_ctx, w_ada[l], b_ada[l])
        h_lat = _modulate(_rms_norm(x, norm_mix_g[l]), m_lat[0], m_lat[1])
        h_ctx = _modulate(_rms_norm(ctx, norm_mix_g[l]), m_ctx[0], m_ctx[1])
        y_lat, y_ctx = _token_mixers(h_lat @ w_in[l], h_ctx @ w_in[l], cos, sin, q_norm_g[l], k_norm_g[l],
                                     attn_sink[l], gla_gate_w[l], gla_gate_b[l], gla_norm_g[l], conv_w[l],
                                     not last)
        x = x + m_lat[2] * (y_lat @ w_out[l])
        h_lat = _modulate(_rms_norm(x, norm_ffn_g[l]), m_lat[3], m_lat[4])
        if last:
            x = x + m_lat[5] * _moe(h_lat, w_router, b_router, w_gate_e[l], w_up_e[l], w_down_e[l])
        else:
            ctx = ctx + m_ctx[2] * (y_ctx @ w_out[l])
            h_ctx = _modulate(_rms_norm(ctx, norm_ffn_g[l]), m_ctx[3], m_ctx[4])
            f = _moe(jnp.concatenate([h_ctx, h_lat], axis=1), w_router, b_router,
                     w_gate_e[l], w_up_e[l], w_down_e[l])
            ctx = ctx + m_ctx[5] * f[:, :ctx_len]
            x = x + m_lat[5] * f[:, ctx_len:]
    return x
```

```python
import contextlib
import numpy as np
import concourse.bass as bass
import concourse.mybir as mybir
from concourse.bass_utils import run_bass_kernel_spmd

F32 = mybir.dt.float32
BF16 = mybir.dt.bfloat16
AF = mybir.ActivationFunctionType
ALU = mybir.AluOpType
AX = mybir.AxisListType

NCORES = 8
NB = 4
D = 1024
S = 2048
L = 256
T = L + S
NT = T // 128
BLKS = [(0, 256), (256, 768), (768, 1280), (1280, 1792), (1792, 2304)]
EPS = 1e-6
N_IN = 2336
NEXP = 16
GLA_CUT = 0


def _dsize(dt):
    return 4 if dt == F32 else 2


def _auto_keys(name, ap, G):
    s = _dsize(ap.dtype)
    dims = ap.ap
    pstep = dims[0][0]
    base = ap.offset % pstep if pstep > 0 else ap.offset
    free = dims[1:]
    offs = [0]
    for (st, n) in free[:-1]:
        if st == 0:
            continue
        offs = [o + st * i for o in offs for i in range(n)]
    lst, ln = free[-1]
    span = abs(lst) * (ln - 1) + 1
    keys = set()
    for o in offs:
        b0 = (base + o) * s
        b1 = (base + o + span) * s - 1
        for g in range(b0 // G, b1 // G + 1):
            keys.add((name, g))
    return tuple(keys)


class V:
    def __init__(self, ap, keys, excl=False):
        self.ap = ap
        self.keys = tuple(keys)
        self.excl = excl

    def __getitem__(self, idx):
        return V(self.ap[idx], self.keys, self.excl)

    def re(self, pat, **kw):
        return V(self.ap.rearrange(pat, **kw), self.keys, self.excl)

    def bc(self, axis, shape):
        return V(self.ap.unsqueeze(axis).to_broadcast(list(shape)), self.keys, self.excl)

    def bcast(self, shape):
        return V(self.ap.to_broadcast(list(shape)), self.keys, self.excl)


class T_:
    def __init__(self, ap, name, G, excl=False):
        self.ap = ap
        self.name = name
        self.G = G
        self.excl = excl

    def __getitem__(self, idx):
        a = self.ap[idx]
        return V(a, _auto_keys(self.name, a, self.G), self.excl)

    def all(self):
        return V(self.ap, _auto_keys(self.name, self.ap, self.G), self.excl)


def DR(ap):
    return V(ap, ())


class Prog:
    ENGS = ("pe", "act", "dve", "pool", "sp")

    def __init__(self, nc):
        self.nc = nc
        self.es = contextlib.ExitStack()
        self.ops = []
        self.last_w = {}
        self.readers = {}
        self.tag_cum = {}
        self.tag_sem = {}
        self.eng_sem = {}
        for e in self.ENGS:
            self.eng_sem[e] = self.es.enter_context(nc.semaphore("s_" + e))

    def sbuf(self, name, shape, dtype, G=1024):
        t = self.es.enter_context(self.nc.sbuf_tensor(name, list(shape), dtype))
        return T_(t[:], name, G)

    def psum(self, name, shape, dtype=F32, G=2048):
        t = self.es.enter_context(self.nc.psum_tensor(name, list(shape), dtype))
        return T_(t[:], name, G, excl=True)

    def _tag(self, tag):
        if tag not in self.tag_sem:
            self.tag_sem[tag] = self.es.enter_context(self.nc.semaphore("d_" + str(tag)))
            self.tag_cum[tag] = 0
        return self.tag_sem[tag]

    def op(self, eng, fn, reads=(), writes=(), dma_tag=None):
        idx = len(self.ops)
        rk = [k for v in reads if not v.excl for k in v.keys]
        wk = [k for v in writes for k in v.keys] + [k for v in reads if v.excl for k in v.keys]
        deps = set()
        for k in rk + wk:
            w = self.last_w.get(k)
            if w is not None:
                deps.add(w)
        for k in wk:
            r = self.readers.get(k)
            if r:
                deps.update(r.values())
        waits = []
        for d in deps:
            Dd = self.ops[d]
            if Dd["dma_tag"] is not None:
                waits.append(("dma", Dd["dma_tag"], self.tag_cum[Dd["dma_tag"]]))
            else:
                if Dd["eng"] == eng and eng == "pe":
                    continue
                Dd["flag"] = True
                waits.append(("eng", Dd["eng"], d))
        if dma_tag is not None:
            self._tag(dma_tag)
            self.tag_cum[dma_tag] += 16
        self.ops.append(dict(eng=eng, fn=fn, waits=waits, dma_tag=dma_tag, flag=False, count=None))
        wks = set(wk)
        for k in wks:
            self.last_w[k] = idx
            self.readers[k] = {}
        for k in rk:
            if k not in wks:
                rkey = eng if dma_tag is None else ("dma", idx)
                self.readers.setdefault(k, {})[rkey] = idx
        return idx

    def wait_all_dma(self, eng, tag):
        self.ops.append(dict(eng=eng, fn=None, waits=[("dma", tag, self.tag_cum[tag])], dma_tag=None,
                             flag=False, count=None))

    def dma(self, eng, out, in_, tag):
        return self.op(eng, lambda e: e.dma_start(out=out.ap, in_=in_.ap), reads=[in_], writes=[out], dma_tag=tag)

    def mm(self, out, lhsT, rhs, start=True, stop=True):
        return self.op("pe", lambda e: e.matmul(out.ap, lhsT.ap, rhs.ap, start=start, stop=stop),
                       reads=[lhsT, rhs], writes=[out])

    def transpose(self, out, in_, ident):
        return self.op("pe", lambda e: e.transpose(out.ap, in_.ap, ident.ap), reads=[in_, ident], writes=[out])

    def act(self, out, in_, func, bias=None, scale=None):
        reads = [in_]
        kw = {}
        if bias is not None:
            if isinstance(bias, V):
                reads.append(bias)
                kw["bias"] = bias.ap
            else:
                kw["bias"] = bias
        if scale is not None:
            if isinstance(scale, V):
                reads.append(scale)
                kw["scale"] = scale.ap
            else:
                kw["scale"] = scale
        return self.op("act", lambda e: e.activation(out.ap, in_.ap, func, **kw), reads=reads, writes=[out])

    def tt(self, out, a, b, op, eng="dve"):
        return self.op(eng, lambda e: e.tensor_tensor(out.ap, a.ap, b.ap, op), reads=[a, b], writes=[out])

    def ts(self, out, a, s1, op0, s2=None, op1=None, eng="dve"):
        reads = [a]
        x1 = s1
        if isinstance(s1, V):
            reads.append(s1)
            x1 = s1.ap
        x2 = s2
        if isinstance(s2, V):
            reads.append(s2)
            x2 = s2.ap
        if op1 is None:
            return self.op(eng, lambda e: e.tensor_scalar(out.ap, a.ap, x1, None, op0), reads=reads, writes=[out])
        return self.op(eng, lambda e: e.tensor_scalar(out.ap, a.ap, x1, x2, op0, op1), reads=reads, writes=[out])

    def stt(self, out, a, s, b, op0, op1, eng="dve"):
        reads = [a, b]
        x = s
        if isinstance(s, V):
            reads.append(s)
            x = s.ap
        return self.op(eng, lambda e: e.scalar_tensor_tensor(out.ap, a.ap, x, b.ap, op0, op1),
                       reads=reads, writes=[out])

    def copy(self, out, in_, eng="dve"):
        if eng == "act":
            return self.op(eng, lambda e: e.copy(out.ap, in_.ap), reads=[in_], writes=[out])
        return self.op(eng, lambda e: e.tensor_copy(out.ap, in_.ap), reads=[in_], writes=[out])

    def memset(self, out, val, eng="dve"):
        return self.op(eng, lambda e: e.memset(out.ap, val), reads=[], writes=[out])

    def reduce(self, out, in_, op, eng="dve"):
        return self.op(eng, lambda e: e.tensor_reduce(out.ap, in_.ap, AX.X, op), reads=[in_], writes=[out])

    def recip(self, out, in_):
        return self.op("dve", lambda e: e.reciprocal(out.ap, in_.ap), reads=[in_], writes=[out])

    def emit(self):
        nc = self.nc
        cnt = {e: 0 for e in self.ENGS}
        for o in self.ops:
            if o["dma_tag"] is None and o["flag"]:
                cnt[o["eng"]] += 1
                o["count"] = cnt[o["eng"]]
        engobj = {"pe": "tensor", "act": "scalar", "dve": "vector", "pool": "gpsimd", "sp": "sync"}
        stats = {e: [0, 0] for e in self.ENGS}
        with nc.Block() as block:
            for e in self.ENGS:
                mine = [o for o in self.ops if o["eng"] == e]

                def body(eng, mine=mine, e=e):
                    waited = {}
                    for o in mine:
                        need = {}
                        for w in o["waits"]:
                            if w[0] == "dma":
                                sem = self.tag_sem[w[1]]
                                val = w[2]
                                key = ("d", w[1])
                            else:
                                sem = self.eng_sem[w[1]]
                                val = self.ops[w[2]]["count"]
                                key = ("e", w[1])
                            if key not in need or need[key][1] < val:
                                need[key] = (sem, val)
                        for key, (sem, val) in need.items():
                            if waited.get(key, 0) < val:
                                eng.wait_ge(sem, val)
                                waited[key] = val
                                stats[e][1] += 1
                        if o["fn"] is None:
                            continue
                        ins = o["fn"](eng)
                        stats[e][0] += 1
                        if o["dma_tag"] is not None:
                            ins.then_inc(self.tag_sem[o["dma_tag"]], 16)
                        elif o["flag"]:
                            ins.then_inc(self.eng_sem[e], 1)

                getattr(block, engobj[e])(body)
        return stats

    def close(self):
        self.es.close()


class Ring:
    def __init__(self, items):
        self.items = items
        self.i = 0

    def next(self):
        x = self.items[self.i % len(self.items)]
        self.i += 1
        return x


class Arena:
    def __init__(self, P, name, nbytes, G=1024):
        self.t = P.es.enter_context(P.nc.sbuf_tensor(name, [128, nbytes // 2], BF16))
        self.name = name
        self.G = G
        self.nbytes = nbytes

    def view(self, off, shape, dtype, parts=128):
        n = 1
        for s in shape:
            n *= s
        nb = n * _dsize(dtype)
        assert off % 4 == 0 and off + nb <= self.nbytes, (off, nb, self.nbytes)
        ap = self.t[0:parts, off // 2:(off + nb) // 2]
        if dtype == F32:
            ap = ap.bitcast(F32)
        if len(shape) == 2:
            ap = ap.rearrange("p (a b) -> p a b", a=shape[0])
        elif len(shape) == 3:
            ap = ap.rearrange("p (a b c) -> p a b c", a=shape[0], b=shape[1])
        elif len(shape) == 4:
            ap = ap.rearrange("p (a b c d) -> p a b c d", a=shape[0], b=shape[1], c=shape[2])
        return T_(ap, self.name, self.G)


def build_program(nb=NB, nlayers=2, dbg_stage=None):
    nc = bass.Bass("TRN2", target_bir_lowering=False)
    P = Prog(nc)

    def din(name, shape, dt=F32):
        return nc.dram_tensor(name, list(shape), dt, kind="ExternalInput").ap()

    xT_d = din("xT", [nb, 128, 8, T])
    cT_d = din("cT", [128, 8, 5])
    wada_d = din("w_ada_r", [2, 48, 128, 8, 128])
    bada_d = din("b_adaT", [128, 2, 48])
    gnorm_d = din("g_normT", [128, 2, 2, 8])
    wfm_d = din("w_fm", [2, 15, 128, 8, 128])
    wtm_d = din("w_tm", [2, 128, 8, 768])
    qkg_d = din("qkg", [128, 2, 2])
    cos_d = din("cosT", [128, T])
    sin_d = din("sinT", [128, T])
    sink_d = din("sink", [128, 16])
    gw_d = din("gla_gw", [128, 2, 256])
    gng_d = din("gla_ng", [128, 2, 64])
    cw_d = din("conv_wT", [128, 2, 2, 3])
    wout_d = din("w_out_r", [2, 128, 8, 1024])
    wr_d = din("w_router_r", [128, 8, 16])
    br_d = din("b_router_r", [128, 16])
    wg_d = din("wg_r", [2, NEXP, 128, 8, 512])
    wu_d = din("wu_r", [2, NEXP, 128, 8, 512])
    wd_d = din("wd_r", [2, NEXP, 128, 4, 1024])
    cst_d = din("consts", [128, 12, 128])
    bdm_d = din("bdmask", [128, 256])
    sel_d = din("sel", [128, 16, 128])
    out_d = nc.dram_tensor("outT", [nb, 128, 8, S], F32, kind="ExternalOutput").ap()

    XT = P.sbuf("XT", [128, 8, T], F32, G=1024)
    HT = P.sbuf("HT", [128, 8, T], BF16, G=512)
    U = Arena(P, "U", 65536, G=256)
    U2 = Arena(P, "U2", 6144, G=256)
    IDENT = P.sbuf("IDENT", [128, 128], F32)
    CB16 = P.sbuf("CB16", [128, 8, 128], BF16, G=256)
    (C_ONES, C_LINC, C_USTR, C_UINC, C_LSTR, C_PERM, C_BD64, C_IDB) = range(8)
    HMASK = P.sbuf("HMASK", [128, 8], F32)
    BDM = P.sbuf("BDM", [128, 256], F32)
    MOD = P.sbuf("MOD", [128, 2, 48, 5], F32, G=64)
    GS = P.sbuf("GS", [128, 2, 2, 8, 5], F32, G=64)
    GN = P.sbuf("GN", [128, 2, 2, 8], F32, G=64)
    BADA = P.sbuf("BADA", [128, 2, 48], F32, G=64)
    CT = P.sbuf("CT", [128, 8, 5], F32)
    QKG = P.sbuf("QKG", [128, 2, 2], F32)
    CW = P.sbuf("CW", [128, 2, 2, 3], F32)
    GNG = P.sbuf("GNG", [128, 2, 64], F32)
    WR32 = P.sbuf("WR32", [128, 8, 16], F32)
    BRT = P.sbuf("BRT", [128, 16], F32, G=64)
    SKE = P.sbuf("SKE", [128, 16], F32, G=64)
    GWB = P.sbuf("GWB", [128, 2, 256], BF16)
    GP = P.sbuf("GP", [128, 4, 128], F32, G=2048)
    SQ = [P.sbuf("SQ%d" % i, [128, 512], BF16) for i in range(2)]
    RS = P.sbuf("RS", [128, 512], F32, G=2048)
    TMPF = [P.sbuf("TMPF%d" % i, [128, 512], F32, G=2048) for i in range(2)]
    HF = [U.view(45568 + i * 2048, [512], F32) for i in range(2)]
    SMALL = P.sbuf("SMALL", [128, 16, 64], F32, G=256)
    PS = [P.psum("PS%d" % i, [128, 512], F32) for i in range(7)]
    PT = P.psum("PT", [128, 1024], BF16)

    sqr = Ring(SQ)
    tmpr = Ring(TMPF)
    hfr = Ring(HF)

    def cst(i):
        return CB16[:, i, :]

    P.dma("sp", IDENT.all(), DR(cst_d[:, 11, :]), "c0")
    P.dma("pool", CB16[:, 0:8, :], DR(cst_d[:, 0:8, :]), "c1")
    P.dma("sp", HMASK.all(), DR(cst_d[:, 8, 0:8]), "c0")
    P.dma("sp", BDM.all(), DR(bdm_d), "c0")
    P.dma("sp", BADA.all(), DR(bada_d), "c0")
    P.dma("sp", GN.all(), DR(gnorm_d), "c0")
    P.dma("sp", CT.all(), DR(cT_d), "c0")
    P.dma("sp", QKG.all(), DR(qkg_d), "c0")
    P.dma("sp", CW.all(), DR(cw_d), "c0")
    P.dma("sp", GNG.all(), DR(gng_d), "c0")
    P.dma("sp", WR32.all(), DR(wr_d), "c0")
    P.dma("sp", BRT.all(), DR(br_d), "c0")
    P.dma("sp", SKE.all(), DR(sink_d), "c0")
    P.dma("pool", GWB.all(), DR(gw_d), "c1")
    P.memset(GP.all(), 0.0)
    P.act(SKE.all(), SKE.all(), AF.Exp)

    SC = P.sbuf("SC", [128, 8, 5], F32)
    P.act(SC.all(), CT.all(), AF.Exp, scale=-1.0)
    P.ts(SC.all(), SC.all(), 1.0, ALU.add)
    P.recip(SC.all(), SC.all())
    P.tt(SC.all(), SC.all(), CT.all(), ALU.mult)
    WA = [U.view(i * 4096, [8, 128], F32) for i in range(3)]
    war = Ring(WA)
    wa_tags = Ring(["wa0", "wa1", "wa2"])
    for l in range(nlayers):
        slots = {}

        def ld(n):
            w = war.next()
            P.dma("sp", w.all(), DR(wada_d[l, n]), wa_tags.next())
            slots[n] = w

        ld(0)
        ld(1)
        for n in range(48):
            if n + 2 < 48:
                ld(n + 2)
            w = slots.pop(n)
            for k in range(8):
                P.mm(PS[0][:, 0:5], w[:, k, :], SC[:, k, :], start=(k == 0), stop=(k == 7))
            P.act(MOD[:, l, n, :], PS[0][:, 0:5], AF.Identity, bias=BADA[:, l, n:n + 1], scale=1.0)
        for s in range(2):
            for c in range(8):
                P.ts(GS[:, l, s, c, :], MOD[:, l, (3 * s + 1) * 8 + c, :], 1.0, ALU.add,
                     GN[:, l, s, c:c + 1], ALU.mult)

    RBANK = [PS[1], PS[4], PS[5], PS[6]]

    def modv(l, kind, c, row):
        return MOD[:, l, kind * 8 + c, row:row + 1]

    def norm_phase(l, s, j, blks, router=None):
        for bi in blks:
            t0, t1 = BLKS[bi]
            n = t1 - t0
            row = 4 if bi == 0 else j
            for c in range(8):
                q = sqr.next()
                P.act(q[:, 0:n], XT[:, c, t0:t1], AF.Square)
                P.mm(PS[0][:, 0:n], cst(C_ONES), q[:, 0:n], start=(c == 0), stop=(c == 7))
            P.act(RS[:, 0:n], PS[0][:, 0:n], AF.Ln, bias=EPS, scale=1.0 / D)
            P.act(RS[:, 0:n], RS[:, 0:n], AF.Exp, scale=-0.5)
            for c in range(8):
                tm = tmpr.next()
                P.stt(tm[:, 0:n], XT[:, c, t0:t1], GS[:, l, s, c, row:row + 1], RS[:, 0:n], ALU.mult, ALU.mult)
                P.act(HT[:, c, t0:t1], tm[:, 0:n], AF.Identity, bias=modv(l, 3 * s, c, row), scale=1.0)
                if router is not None:
                    hf = hfr.next()
                    P.ts(hf[:, 0:n], tm[:, 0:n], modv(l, 3 * s, c, row), ALU.add)
                    for q4 in range(n // 128):
                        P.mm(RBANK[q4][:, 0:16], hf[:, q4 * 128:(q4 + 1) * 128], WR32[:, c, :],
                             start=(c == 0), stop=(c == 7))
            if router is not None:
                router(bi, n // 128)

    wfm_slots = [U.view(57344 + i * 2048, [8, 128], BF16) for i in range(2)]
    wfm_ring = Ring(wfm_slots)
    wfm_tags = Ring(["wf0", "wf1"])

    def load_wfm(l, ch):
        w = wfm_ring.next()
        P.dma("pool", w.all(), DR(wfm_d[l, ch]), wfm_tags.next())
        return w

    projps = Ring([PS[2], PS[3]])

    def proj_fm(w, bi):
        t0, t1 = BLKS[bi]
        ps = projps.next()
        for k in range(8):
            P.mm(ps[:, 0:t1 - t0], w[:, k, :], HT[:, k, t0:t1], start=(k == 0), stop=(k == 7))
        return ps

    WO = U.view(53248, [2, 1024], BF16)

    def wout_partial(l, j, kc0, YT, blks):
        P.dma("pool", WO.all(), DR(wout_d[l, :, kc0:kc0 + 2, :]), "wo")
        for bi in blks:
            t0, t1 = BLKS[bi]
            n = t1 - t0
            row = 4 if bi == 0 else j
            for d in range(8):
                ps = projps.next()
                for k in range(2):
                    P.mm(ps[:, 0:n], WO[:, k, d * 128:(d + 1) * 128], YT[:, k, t0:t1], start=(k == 0), stop=(k == 1))
                P.stt(XT[:, d, t0:t1], ps[:, 0:n], modv(l, 2, d, row), XT[:, d, t0:t1], ALU.mult, ALU.add)

    def conv_group(l, j, blks_out):
        CBf = U.view(0, [T], F32)
        CCf = U.view(9216, [T], F32)
        UU = U.view(18432, [T], F32)
        ACC = U.view(27648, [T], F32)
        YT = U.view(36864, [2, T], BF16)
        for cc in range(2):
            for which, dst in ((9 + cc, CBf), (11 + cc, CCf), (13 + cc, None)):
                w = load_wfm(l, which)
                for bi in range(5):
                    t0, t1 = BLKS[bi]
                    ps = proj_fm(w, bi)
                    if dst is not None:
                        P.copy(dst[:, t0:t1], ps[:, 0:t1 - t0], eng="act")
                    else:
                        P.tt(UU[:, t0:t1], ps[:, 0:t1 - t0], CCf[:, t0:t1], ALU.mult)
            P.ts(ACC.all(), UU.all(), CW[:, l, cc, 1:2], ALU.mult)
            for (a, b) in ((0, L), (L, T)):
                P.stt(ACC[:, a + 1:b], UU[:, a:b - 1], CW[:, l, cc, 0:1], ACC[:, a + 1:b], ALU.mult, ALU.add)
                P.stt(ACC[:, a:b - 1], UU[:, a + 1:b], CW[:, l, cc, 2:3], ACC[:, a:b - 1], ALU.mult, ALU.add)
            P.tt(YT[:, cc, :], CBf.all(), ACC.all(), ALU.mult)
        wout_partial(l, j, 6, YT, blks_out)

    def attn_group(l, j, kv, with_ctx_out):
        QT = U.view(0, [2, T], BF16)
        KM = U.view(9216, [2, T], BF16)
        VX = U.view(18432, [NT, 128], BF16)
        YT = U.view(23040, [2, T], BF16)
        COS = U.view(32256, [T], BF16)
        SIN = U.view(36864, [T], BF16)
        KN = U.view(41472, [512], BF16)
        QN = U.view(42496, [512], BF16)
        SQB = U.view(43520, [512], BF16)
        EX = [U.view(44544 + i * 1024, [512], BF16) for i in range(3)]
        RD = U.view(47616, [512], F32)
        WV = U.view(49664, [8, 64], BF16)
        RQ = U.view(50688, [512], F32)
        ESK = U.view(61440, [512], F32)
        DEN = U.view(63488, [512], F32)
        exr = Ring(EX)
        sqbr = Ring([SQB, U2.view(0, [512], BF16)])
        qnr = Ring([QN, U2.view(1024, [512], BF16)])
        knr = Ring([KN, U2.view(2048, [512], BF16)])
        rqr = Ring([RQ, U2.view(3072, [512], F32)])
        denr = Ring([DEN, U2.view(0, [512], F32)])
        rdr = Ring([RD, U2.view(2048, [512], F32)])
        ssps = Ring([PS[4], PS[0]])
        rotps = Ring([PS[5], PS[1]])
        P.dma("pool", COS.all(), DR(cos_d), "tab")
        P.dma("pool", SIN.all(), DR(sin_d), "tab")
        P.dma("pool", WV.all(), DR(wtm_d[l, :, :, kv * 64:(kv + 1) * 64]), "wv")
        for a in range(2):
            for b in range(2):
                idx = 8 * l + 4 * kv + 2 * b + a
                P.copy(ESK[:, (2 * a + b) * 128:(2 * a + b + 1) * 128], SKE[:, idx:idx + 1].bcast([128, 128]), eng="act")
        for which in (2 * kv, 2 * kv + 1, 4 + kv):
            isk = which >= 4
            w = load_wfm(l, which)
            for bi in range(5):
                t0, t1 = BLKS[bi]
                n = t1 - t0
                ps = proj_fm(w, bi)
                sqb = sqbr.next()
                rq = rqr.next()
                qn = qnr.next()
                ssp = ssps.next()
                rtp = rotps.next()
                P.act(sqb[:, 0:n], ps[:, 0:n], AF.Square)
                P.mm(ssp[:, 0:n], cst(C_BD64), sqb[:, 0:n])
                P.act(rq[:, 0:n], ssp[:, 0:n], AF.Ln, bias=EPS, scale=1.0 / 64)
                P.act(rq[:, 0:n], rq[:, 0:n], AF.Exp, scale=-0.5)
                P.stt(qn[:, 0:n], ps[:, 0:n], QKG[:, l, (1 if isk else 0):(2 if isk else 1)], rq[:, 0:n],
                      ALU.mult, ALU.mult)
                P.mm(rtp[:, 0:n], cst(C_PERM), qn[:, 0:n])
                t1_ = tmpr.next()
                t2_ = tmpr.next()
                P.tt(t1_[:, 0:n], qn[:, 0:n], COS[:, t0:t1], ALU.mult)
                P.tt(t2_[:, 0:n], rtp[:, 0:n], SIN[:, t0:t1], ALU.mult)
                if not isk:
                    P.tt(QT[:, which - 2 * kv, t0:t1], t1_[:, 0:n], t2_[:, 0:n], ALU.add)
                else:
                    kn = knr.next()
                    P.tt(kn[:, 0:n], t1_[:, 0:n], t2_[:, 0:n], ALU.add)
                    P.ts(KM[:, 0, t0:t1], kn[:, 0:n], HMASK[:, 0:1], ALU.mult)
                    P.ts(KM[:, 1, t0:t1], kn[:, 0:n], HMASK[:, 1:2], ALU.mult)
        vps = Ring([PS[4], PS[6]])
        for i in range(NT):
            vp = vps.next()
            for k in range(8):
                P.mm(vp[:, 0:64], HT[:, k, i * 128:(i + 1) * 128], WV[:, k, :], start=(k == 0), stop=(k == 7))
            P.copy(VX[:, i, 0:64], vp[:, 0:64], eng="act")
            P.copy(VX[:, i, 64:128], vp[:, 0:64], eng="act")
        stps = Ring([PS[0], PS[1]])
        numr = Ring([PS[2], PS[3]])
        dnr = Ring([PS[4], PS[5]])
        qtiles = list(range(2, NT)) + ([0, 1] if with_ctx_out else [])
        its = []
        for i in qtiles:
            if i >= 2:
                keys = [(0, None), (1, None)]
                if i - 1 >= 2:
                    keys.append((i - 1, C_UINC))
                keys.append((i, None))
                if i + 1 < NT:
                    keys.append((i + 1, C_LINC))
            else:
                keys = [(0, None), (1, None)]
            for ki, (kt, msk) in enumerate(keys):
                its.append((i, kt, msk, ki == 0, ki == len(keys) - 1))

        def scores(it):
            i, kt, msk, first, lastk = it
            st = stps.next()
            for par in range(2):
                P.mm(st[:, par * 256:(par + 1) * 256].re("p (c q) -> p c q", c=2),
                     KM[:, par, kt * 128:(kt + 1) * 128], QT[:, :, i * 128:(i + 1) * 128])
            return st

        cur = {}
        st_next = scores(its[0])
        for n_, it in enumerate(its):
            i, kt, msk, first, lastk = it
            st = st_next
            if n_ + 1 < len(its):
                st_next = scores(its[n_ + 1])
            if first:
                cur["num"] = numr.next()
                cur["den"] = dnr.next()
            ex = exr.next()
            P.act(ex.all(), st.all(), AF.Exp, scale=0.125)
            if msk is not None:
                P.tt(ex.all().re("p (h q) -> p h q", h=4), ex.all().re("p (h q) -> p h q", h=4),
                     cst(msk).bc(1, [128, 4, 128]), ALU.mult)
            P.mm(cur["num"].all(), VX[:, kt, :], ex.all(), start=first, stop=lastk)
            P.mm(cur["den"].all(), cst(C_ONES), ex.all(), start=first, stop=lastk)
            if lastk:
                den = denr.next()
                rd = rdr.next()
                P.tt(den.all(), cur["den"].all(), ESK.all(), ALU.add)
                P.act(rd.all(), den.all(), AF.Ln)
                P.act(rd.all(), rd.all(), AF.Exp, scale=-1.0)
                for par in range(2):
                    for cc in range(2):
                        cb = (2 * par + cc) * 128
                        P.tt(YT[par * 64:(par + 1) * 64, cc, i * 128:(i + 1) * 128],
                             cur["num"][par * 64:(par + 1) * 64, cb:cb + 128],
                             rd[par * 64:(par + 1) * 64, cb:cb + 128], ALU.mult)
        wout_partial(l, j, 2 * kv, YT, [0, 1, 2, 3, 4] if with_ctx_out else [1, 2, 3, 4])

    def gla_group(l, j, with_ctx_out):
        GQ = U.view(0, [T], BF16)
        GK = U.view(4608, [T], BF16)
        GLW = U.view(9216, [T], BF16)
        KTOK = U.view(13824, [NT, 128], BF16)
        VTOK = U.view(18432, [NT, 256], BF16)
        OF = U.view(27648, [NT, 256], BF16)
        YT = U.view(36864, [2, T], BF16)
        WRG = U.view(46080, [8, 256], BF16)
        WKV = U.view(50176, [8, 384], BF16)
        GALL = U.view(50176, [NT, 256], BF16)
        yb = U2.view(5632, [256], BF16)
        EB, ENB, ESUF = gla_f32[0], gla_f32[1], gla_f32[2]
        SETS = {
            (0, 0): dict(QB=U.view(61440, [128], BF16), KD=U.view(61696, [128], BF16), KB4=U.view(61952, [4, 128], BF16)),
            (0, 1): dict(QB=U2.view(0, [128], BF16), KD=U2.view(256, [128], BF16), KB4=U2.view(512, [4, 128], BF16)),
            (1, 0): dict(QB=U2.view(1536, [128], BF16), KD=U2.view(1792, [128], BF16), KB4=U2.view(2048, [4, 128], BF16)),
            (1, 1): dict(QB=U2.view(3072, [128], BF16), KD=U2.view(3328, [128], BF16), KB4=U2.view(3584, [4, 128], BF16)),
        }
        ATMS = [U.view(62976, [4, 128], BF16), U2.view(4608, [4, 128], BF16)]
        SBF = [U.view(64256 + d * 512, [256], BF16) for d in range(2)]
        S32 = [gla_s32[0], gla_s32[1]]
        KVMS = [gla_s32[2], gla_s32[3]]
        ATT_PS = [PS[0], PS[1]]
        bps = Ring([PS[4], PS[5]])

        P.dma("pool", WKV.all(), DR(wtm_d[l, :, :, 128:512]), "wkv")
        P.dma("pool", WRG.all(), DR(wtm_d[l, :, :, 512:768]), "wrg")
        for which, dst in ((6, GQ), (7, GK), (8, GLW)):
            w = load_wfm(l, which)
            for bi in range(5):
                t0, t1 = BLKS[bi]
                ps = proj_fm(w, bi)
                if which == 6:
                    P.act(dst[:, t0:t1], ps[:, 0:t1 - t0], AF.Identity, scale=32.0 ** -0.5, bias=0.0)
                else:
                    P.copy(dst[:, t0:t1], ps[:, 0:t1 - t0], eng="act")
        P.ts(GLW.all(), GLW.all(), HMASK[:, 6:7], ALU.add)
        kvr = Ring([(PS[0], PS[1]), (PS[2], PS[3])])
        for i in range(NT):
            pk, pv = kvr.next()
            for k in range(8):
                P.mm(pk[:, 0:128], HT[:, k, i * 128:(i + 1) * 128], WKV[:, k, 0:128], start=(k == 0), stop=(k == 7))
            for k in range(8):
                P.mm(pv[:, 0:256], HT[:, k, i * 128:(i + 1) * 128], WKV[:, k, 128:384], start=(k == 0), stop=(k == 7))
            P.copy(KTOK[:, i, :], pk[:, 0:128], eng="act")
            P.copy(VTOK[:, i, :], pv[:, 0:256], eng="dve")
        zr = Ring([PS[5], PS[4]])
        for i in range(NT):
            zp = zr.next()
            P.mm(zp[:, 0:256], GLW[:, i * 128:(i + 1) * 128], GWB[:, l, :])
            e1 = tmpr.next()
            P.act(e1[:, 0:256], zp[:, 0:256], AF.Exp, scale=-1.0)
            P.act(e1[:, 0:256], e1[:, 0:256], AF.Ln, bias=1.0, scale=1.0)
            P.ts(GALL[:, i, :], e1[:, 0:256], -1.0 / 16.0, ALU.mult)

        order = [list(range(NT)), [1, 0] + list(range(NT - 1, 1, -1))]
        stepof = [{i: n for n, i in enumerate(order[d])} for d in range(2)]

        def stage_b(n, d):
            i = order[d][n]
            need_out = with_ctx_out or i >= 2
            X = SETS[(n % 2, d)]
            Mcum, Msuf = (C_LINC, C_USTR) if d == 0 else (C_UINC, C_LSTR)
            tcol = 127 if d == 0 else 0
            sl = slice(i * 128, (i + 1) * 128)
            g = GALL[:, i, d * 128:(d + 1) * 128]
            bp = bps.next()
            P.mm(bp[:, 0:128], g, cst(Mcum))
            P.mm(bp[:, 128:256], cst(Msuf), g)
            P.act(EB.all(), bp[:, 0:128], AF.Exp)
            P.act(ESUF.all(), bp[:, 128:256], AF.Exp)
            if need_out:
                P.act(ENB.all(), bp[:, 0:128], AF.Exp, scale=-1.0)
            slot = 2 * (n % 2) + d
            P.copy(SMALL[:, 15, slot:slot + 1], EB[:, tcol:tcol + 1], eng="act")
            P.tt(X["KD"].all(), KTOK[:, i, :], ESUF.all(), ALU.mult)
            if need_out:
                P.tt(X["QB"].all(), GQ[:, sl], EB.all(), ALU.mult)
                P.tt(ENB.all(), GK[:, sl], ENB.all(), ALU.mult)
                for h in range(4):
                    P.act(X["KB4"][:, h, :], ENB.all(), AF.Identity, scale=HMASK[:, 2 + h:3 + h], bias=0.0)

        def stage_c(n):
            tiles = [order[d][n] for d in range(2)]
            needs = [with_ctx_out or tiles[d] >= 2 for d in range(2)]
            finals = [stepof[1 - d][tiles[d]] < n for d in range(2)]
            Xs = [SETS[(n % 2, d)] for d in range(2)]
            for d in range(2):
                if needs[d]:
                    for h in range(4):
                        P.mm(ATT_PS[d][:, h * 128:(h + 1) * 128], Xs[d]["KB4"][:, h, :], Xs[d]["QB"].all())
            for d in range(2):
                P.mm(PS[2][:, 0:256] if d == 0 else PS[3][:, 0:256], Xs[d]["KD"].all(), VTOK[:, tiles[d], :])
            for d in range(2):
                if needs[d]:
                    Mmask = C_LINC if d == 0 else C_USTR
                    P.tt(ATMS[d].all(), ATT_PS[d].all().re("p (h q) -> p h q", h=4),
                         cst(Mmask).bc(1, [128, 4, 128]), ALU.mult)
            for d in range(2):
                slot = 2 * (n % 2) + d
                kvp = PS[2] if d == 0 else PS[3]
                P.tt(KVMS[d].all(), kvp[:, 0:256], BDM.all(), ALU.mult)
                P.stt(S32[d].all(), S32[d].all(), SMALL[:, 15, slot:slot + 1], KVMS[d].all(), ALU.mult, ALU.add)
            for d in range(2):
                if not needs[d]:
                    P.copy(SBF[d].all(), S32[d].all(), eng="act")
                    continue
                i = tiles[d]
                sl = slice(i * 128, (i + 1) * 128)
                for h in range(4):
                    hs = slice(h * 64, (h + 1) * 64)
                    P.mm(PS[6][:, hs], Xs[d]["QB"].all(), SBF[d][:, hs], start=True, stop=False)
                    P.mm(PS[6][:, hs], ATMS[d][:, h, :], VTOK[:, i, hs], start=False, stop=True)
                P.copy(SBF[d].all(), S32[d].all(), eng="act")
                if not finals[d]:
                    P.copy(OF[:, i, :], PS[6][:, 0:256], eng="act")
                    continue
                osum = tmpr.next()
                P.tt(osum[:, 0:256], PS[6][:, 0:256], OF[:, i, :], ALU.add)
                for k in range(8):
                    P.mm(PS[6][:, 256:512], HT[:, k, sl], WRG[:, k, :], start=(k == 0), stop=(k == 7))
                sq = tmpr.next()
                P.tt(sq[:, 0:256], osum[:, 0:256], osum[:, 0:256], ALU.mult)
                ssh = SMALL[:, 0, 0:4]
                P.reduce(ssh, sq[:, 0:256].re("p (h e) -> p h e", h=4), ALU.add)
                P.act(ssh, ssh, AF.Ln, bias=EPS, scale=1.0 / 64)
                P.act(ssh, ssh, AF.Exp, scale=-0.5)
                o3 = osum[:, 0:256].re("p (h e) -> p h e", h=4)
                P.tt(o3, o3, ssh.bc(2, [128, 4, 64]), ALU.mult)
                P.tt(o3, o3, GNG[:, l, :].bc(1, [128, 4, 64]), ALU.mult)
                er = sq
                P.act(er[:, 256:512], PS[6][:, 256:512], AF.Exp, scale=-1.0)
                P.act(er[:, 256:512], er[:, 256:512], AF.Ln, bias=1.0, scale=1.0)
                P.act(er[:, 256:512], er[:, 256:512], AF.Exp, scale=-1.0)
                P.tt(er[:, 256:512], er[:, 256:512], PS[6][:, 256:512], ALU.mult)
                P.tt(yb.all(), osum[:, 0:256], er[:, 256:512], ALU.mult)
                for c2 in range(2):
                    P.transpose(PT[:, c2 * 128:(c2 + 1) * 128], yb[:, c2 * 128:(c2 + 1) * 128], cst(C_IDB))
                P.copy(YT[:, :, sl], PT[:, 0:256].re("p (c q) -> p c q", c=2), eng="act")

        for d in range(2):
            P.memset(S32[d].all(), 0.0)
            P.memset(SBF[d].all(), 0.0)
        stage_b(0, 0)
        stage_b(0, 1)
        for n in range(NT):
            if n + 1 < NT:
                stage_b(n + 1, 0)
                stage_b(n + 1, 1)
            stage_c(n)
        wout_partial(l, j, 4, YT, [0, 1, 2, 3, 4] if with_ctx_out else [1, 2, 3, 4])

    gla_f32 = [P.sbuf("GF%d" % i, [128, 128], F32, G=512) for i in range(4)]
    gla_s32 = [P.sbuf("GS32_%d" % i, [128, 256], F32, G=1024) for i in range(4)]

    def moe_phase(l, j, blks):
        GT = U.view(40960, [T], BF16)
        ABUF = [U.view(45568 + i * 4096, [4, 512], BF16) for i in range(2)]
        SG = [U.view(53760 + i * 1024, [512], BF16) for i in range(2)]
        T1 = [U.view(55808 + i * 1024, [512], BF16) for i in range(2)]
        GBC = [U.view(57856 + i * 1024, [512], BF16) for i in range(2)]
        WSL = [U.view(i * 8192, [8, 512], BF16) for i in range(5)]
        WSLD = [U.view(i * 8192, [4, 1024], BF16) for i in range(5)]
        SEL = U.view(59904, [16, 128], BF16)
        P.dma("pool", SEL.all(), DR(sel_d), "sel")
        wring = Ring(list(range(5)))
        wtags = ["we%d" % i for i in range(5)]
        LG = SMALL

        def router(bi, nt):
            t0, _ = BLKS[bi]
            lgb = SMALL[:, 14, 0:nt * 16]
            lg3 = lgb.re("p (q e) -> p q e", q=nt)
            for q4 in range(nt):
                P.tt(lgb[:, q4 * 16:(q4 + 1) * 16], RBANK[q4][:, 0:16], BRT.all(), ALU.add)
            mx = SMALL[:, 0, 0:nt]
            P.reduce(mx, lg3, ALU.max)
            lgs = SMALL[:, 1, 0:nt * 16]
            P.tt(lgs.re("p (q e) -> p q e", q=nt), lg3, mx.bc(2, [128, nt, 16]), ALU.subtract)
            ex = SMALL[:, 2, 0:nt * 16]
            P.act(ex, lgs, AF.Exp)
            ng = nt * 4
            ex4 = ex.re("p (g f) -> p g f", g=ng)
            m1 = SMALL[:, 3, 0:ng]
            P.reduce(m1, ex4, ALU.max)
            eq = SMALL[:, 4, 0:nt * 16]
            eq4 = eq.re("p (g f) -> p g f", g=ng)
            P.tt(eq4, ex4, m1.bc(2, [128, ng, 4]), ALU.is_equal)
            e2 = SMALL[:, 5, 0:nt * 16]
            P.stt(e2, eq, -1.0e30, ex, ALU.mult, ALU.add)
            m2 = SMALL[:, 6, 0:ng]
            P.reduce(m2, e2.re("p (g f) -> p g f", g=ng), ALU.max)
            gsc = SMALL[:, 7, 0:ng]
            P.tt(gsc, m1, m2, ALU.add)
            gm = SMALL[:, 8, 0:nt]
            P.reduce(gm, gsc.re("p (q g) -> p q g", q=nt), ALU.max)
            gsel = SMALL[:, 9, 0:ng]
            P.tt(gsel.re("p (q g) -> p q g", q=nt), gsc.re("p (q g) -> p q g", q=nt), gm.bc(2, [128, nt, 4]),
                 ALU.is_equal)
            selx = SMALL[:, 10, 0:nt * 16]
            selx4 = selx.re("p (g f) -> p g f", g=ng)
            P.tt(selx4, ex4, m2.bc(2, [128, ng, 4]), ALU.is_ge)
            P.tt(selx4, selx4, gsel.bc(2, [128, ng, 4]), ALU.mult)
            wgt = SMALL[:, 11, 0:nt * 16]
            P.tt(wgt, ex, selx, ALU.mult)
            den = SMALL[:, 12, 0:nt]
            P.reduce(den, wgt.re("p (q e) -> p q e", q=nt), ALU.add)
            P.recip(den, den)
            P.tt(GP[:, 0:nt, 0:16], wgt.re("p (q e) -> p q e", q=nt), den.bc(2, [128, nt, 16]), ALU.mult)
            for q4 in range(nt):
                P.transpose(PS[6][:, q4 * 128:(q4 + 1) * 128], GP[:, q4, :], IDENT.all())
            P.copy(GT[:, t0:t0 + nt * 128], PS[6][:, 0:nt * 128], eng="act")

        norm_phase(l, 1, j, blks, router=router)
        if dbg_stage == "gates":
            P.copy(XT[:, 0, :], GT.all(), eng="act")
            return

        gps = Ring([PS[0], PS[1]])
        ups = Ring([PS[2], PS[3]])
        ops_ = Ring([PS[4], PS[5]])
        abr = Ring(ABUF)
        sgr = Ring(SG)
        t1r = Ring(T1)
        gbr = Ring(GBC)

        def load_gu(e):
            sl = [wring.next() for _ in range(3)]
            P.dma("pool", WSL[sl[0]].all(), DR(wg_d[l, e]), wtags[sl[0]])
            P.dma("pool", WSL[sl[1]].all(), DR(wu_d[l, e]), wtags[sl[1]])
            return sl

        def load_d(e, sl):
            P.dma("pool", WSLD[sl[2]].all(), DR(wd_d[l, e]), wtags[sl[2]])

        def gu_steps(e, bi, sl):
            t0, t1 = BLKS[bi]
            n = t1 - t0
            gb = gbr.next()
            ab = abr.next()

            def pre():
                P.mm(PS[6][:, 0:n], SEL[:, e, :], GT[:, t0:t1])
                P.copy(gb[:, 0:n], PS[6][:, 0:n], eng="act")

            def fstep(f):
                g = gps.next()
                u = ups.next()
                for k in range(8):
                    P.mm(g[:, 0:n], WSL[sl[0]][:, k, f * 128:(f + 1) * 128], HT[:, k, t0:t1], start=(k == 0), stop=(k == 7))
                for k in range(8):
                    P.mm(u[:, 0:n], WSL[sl[1]][:, k, f * 128:(f + 1) * 128], HT[:, k, t0:t1], start=(k == 0), stop=(k == 7))
                sg = sgr.next()
                P.act(sg[:, 0:n], g[:, 0:n], AF.Silu)
                t1_ = t1r.next()
                P.tt(t1_[:, 0:n], u[:, 0:n], sg[:, 0:n], ALU.mult)
                P.tt(ab[:, f, 0:n], t1_[:, 0:n], gb[:, 0:n], ALU.mult)

            return pre, fstep, ab

        def down_step(e, bi, sl, ab, d):
            t0, t1 = BLKS[bi]
            n = t1 - t0
            row = 4 if bi == 0 else j
            wd = WSLD[sl[2]]
            o = ops_.next()
            for f in range(4):
                P.mm(o[:, 0:n], wd[:, f, d * 128:(d + 1) * 128], ab[:, f, 0:n], start=(f == 0), stop=(f == 3))
            P.stt(XT[:, d, t0:t1], o[:, 0:n], modv(l, 5, d, row), XT[:, d, t0:t1], ALU.mult, ALU.add)

        pend = None
        nxt = load_gu(0)
        load_d(0, nxt)
        for e in range(NEXP):
            sl = nxt
            for bi in blks:
                pre, fstep, ab = gu_steps(e, bi, sl)
                pre()
                for f in range(4):
                    fstep(f)
                    if pend is not None:
                        down_step(*pend, 2 * f)
                        down_step(*pend, 2 * f + 1)
                pend = (e, bi, sl, ab)
                if bi == blks[0] and e + 1 < NEXP:
                    nxt = load_gu(e + 1)
            if e + 1 < NEXP:
                load_d(e + 1, nxt)
        for d in range(8):
            down_step(*pend, d)

    for j in range(nb):
        for c in range(8):
            P.dma("sp", XT[:, c, :], DR(xT_d[j, :, c, :]), "xin")
        for l in range(nlayers):
            last = (l == nlayers - 1) and nlayers == 2
            allb = [0, 1, 2, 3, 4]
            latb = [1, 2, 3, 4]
            norm_phase(l, 0, j, allb)
            if dbg_stage == "norm0":
                for c in range(8):
                    P.copy(XT[:, c, :], HT[:, c, :], eng="act")
                break
            conv_group(l, j, latb if last else allb)
            if dbg_stage == "conv":
                break
            gla_group(l, j, not last)
            if dbg_stage == "gla":
                break
            for kv in range(2):
                attn_group(l, j, kv, not last)
            if dbg_stage == "mix":
                break
            moe_phase(l, j, latb if last else allb)
            if dbg_stage == "gates":
                break
        for c in range(8):
            P.dma("sp", DR(out_d[j, :, c, :]), XT[:, c, L:T], "xout")
        if dbg_stage == "norm0":
            pass
    P.wait_all_dma("sp", "xout")
    stats = P.emit()
    P.close()
    return nc, stats


def _rope_tables():
    rows = S // 64
    r, c = np.meshgrid(np.arange(rows), np.arange(64), indexing="ij")
    nf = 16
    inv = (10000.0 ** (-np.arange(nf, dtype=np.float32) / nf)).astype(np.float32)
    ang = np.concatenate([r.reshape(-1, 1).astype(np.float32) * inv, c.reshape(-1, 1).astype(np.float32) * inv], -1)
    cos = np.cos(ang).astype(np.float32)
    sin = np.sin(ang).astype(np.float32)
    cosT = np.ones((128, T), np.float32)
    sinT = np.zeros((128, T), np.float32)
    for p in range(128):
        d = p % 64
        i = d % 32
        cosT[p, L:] = cos[:, i]
        sinT[p, L:] = (-sin[:, i]) if d < 32 else sin[:, i]
    return cosT, sinT


def _consts():
    c = np.zeros((128, 12, 128), np.float32)
    s = np.arange(128)[:, None]
    t = np.arange(128)[None, :]
    c[:, 0] = 1.0
    c[:, 1] = (s <= t)
    c[:, 2] = (s > t)
    c[:, 3] = (s >= t)
    c[:, 4] = (s < t)
    perm = np.zeros((128, 128), np.float32)
    for m in range(128):
        partner = m + 32 if (m % 64) < 32 else m - 32
        perm[partner, m] = 1.0
    c[:, 5] = perm
    c[:, 6] = ((s // 64) == (t // 64))
    c[:, 7] = np.eye(128)
    hm = np.zeros((128, 128), np.float32)
    hm[:64, 0] = 1.0
    hm[64:, 1] = 1.0
    for h in range(4):
        hm[32 * h:32 * h + 32, 2 + h] = 1.0
    hm[32, 6] = 1.0
    c[:, 8] = hm
    c[:, 11] = np.eye(128)
    bdm = ((np.arange(128)[:, None] // 32) == (np.arange(256)[None, :] // 64)).astype(np.float32)
    sel = np.zeros((128, 16, 128), np.float32)
    for e in range(16):
        sel[e, e, :] = 1.0
    return c, bdm, sel


def _prep_weights(inp):
    f = lambda a: np.ascontiguousarray(np.asarray(a, dtype=np.float32))
    w = {}
    w_ada = f(inp["w_ada"])
    w["w_ada_r"] = f(w_ada.reshape(2, 8, 128, 48, 128).transpose(0, 3, 2, 1, 4))
    w["b_adaT"] = f(f(inp["b_ada"]).reshape(2, 48, 128).transpose(2, 0, 1))
    gn = np.stack([f(inp["norm_mix_g"]), f(inp["norm_ffn_g"])], 1)
    w["g_normT"] = f(gn.reshape(2, 2, 8, 128).transpose(3, 0, 1, 2))
    w_in = f(inp["w_in"])
    cols = []
    for c in range(4):
        cols.append(np.arange(c * 128, (c + 1) * 128))
    cols.append(np.concatenate([np.arange(512, 576), np.arange(512, 576)]))
    cols.append(np.concatenate([np.arange(576, 640), np.arange(576, 640)]))
    cols.append(np.arange(768, 896))
    cols.append(np.arange(896, 1024))
    cols.append(None)
    for b0 in (1568, 1824, 2080):
        cols.append(np.arange(b0, b0 + 128))
        cols.append(np.arange(b0 + 128, b0 + 256))
    wfm = np.zeros((2, 15, 1024, 128), np.float32)
    for i, cidx in enumerate(cols):
        if cidx is None:
            wfm[:, i, :, 0:32] = w_in[:, :, 1536:1568]
        else:
            wfm[:, i] = w_in[:, :, cidx]
    w["w_fm"] = f(wfm.reshape(2, 15, 8, 128, 128).transpose(0, 1, 3, 2, 4))
    tmc = np.concatenate([np.arange(640, 768), np.arange(896, 1024), np.arange(1024, 1280), np.arange(1280, 1536)])
    w["w_tm"] = f(w_in[:, :, tmc].reshape(2, 8, 128, 768).transpose(0, 2, 1, 3))
    qg = f(inp["q_norm_g"])
    kg = f(inp["k_norm_g"])
    qkg = np.zeros((128, 2, 2), np.float32)
    for p in range(128):
        qkg[p, :, 0] = qg[:, p % 64]
        qkg[p, :, 1] = kg[:, p % 64]
    w["qkg"] = qkg
    w["cosT"], w["sinT"] = _rope_tables()
    w["sink"] = f(np.broadcast_to(f(inp["attn_sink"]).reshape(1, 16), (128, 16)))
    gw = f(inp["gla_gate_w"])
    gwm = np.zeros((128, 2, 256), np.float32)
    gwm[0:16, :, 0:128] = gw[:, 0].transpose(1, 0, 2)
    gwm[16:32, :, 128:256] = gw[:, 1].transpose(1, 0, 2)
    gwm[32] = f(inp["gla_gate_b"]).reshape(2, 256)
    w["gla_gw"] = gwm
    w["gla_ng"] = f(np.broadcast_to(f(inp["gla_norm_g"])[None], (128, 2, 64)))
    cw = f(inp["conv_w"])
    w["conv_wT"] = f(cw.reshape(2, 3, 2, 128).transpose(3, 0, 2, 1))
    w["w_out_r"] = f(f(inp["w_out"]).reshape(2, 8, 128, 1024).transpose(0, 2, 1, 3))
    w["w_router_r"] = f(f(inp["w_router"]).reshape(8, 128, 16).transpose(1, 0, 2))
    w["b_router_r"] = f(np.broadcast_to(f(inp["b_router"]).reshape(1, 16), (128, 16)))
    w["wg_r"] = f(f(inp["w_gate_e"]).reshape(2, 16, 8, 128, 512).transpose(0, 1, 3, 2, 4))
    w["wu_r"] = f(f(inp["w_up_e"]).reshape(2, 16, 8, 128, 512).transpose(0, 1, 3, 2, 4))
    w["wd_r"] = f(f(inp["w_down_e"]).reshape(2, 16, 4, 128, 1024).transpose(0, 1, 3, 2, 4))
    c, bdm, sel = _consts()
    w["consts"] = c
    w["bdmask"] = bdm
    w["sel"] = sel
    return w


def _prep_core(inp, b0, nb):
    x = np.asarray(inp["x"], dtype=np.float32)[b0:b0 + nb]
    ctx = np.asarray(inp["ctx"], dtype=np.float32)[b0:b0 + nb]
    full = np.concatenate([ctx, x], axis=1)
    xT = np.ascontiguousarray(full.reshape(nb, T, 8, 128).transpose(0, 3, 2, 1))
    c5 = np.zeros((5, D), np.float32)
    cc = np.asarray(inp["c"], dtype=np.float32)[b0:b0 + nb]
    c5[:nb] = cc
    c5[4] = np.asarray(inp["c_ctx"], dtype=np.float32)
    cT = np.ascontiguousarray(c5.reshape(5, 8, 128).transpose(2, 1, 0))
    return {"xT": xT, "cT": cT}


_CACHE = {}


def kernel(**inputs):
    w = _prep_weights(inputs)
    if "nc" not in _CACHE:
        _CACHE["nc"] = build_program(NB, 2)[0]
    nc = _CACHE["nc"]
    in_maps = []
    for core in range(NCORES):
        m = dict(w)
        m.update(_prep_core(inputs, core * NB, NB))
        in_maps.append(m)
    res = run_bass_kernel_spmd(nc, in_maps, core_ids=list(range(NCORES)))
    outs = []
    for core in range(NCORES):
        o = np.asarray(res.results[core]["outT"])
        outs.append(o.transpose(0, 3, 2, 1).reshape(NB, S, D))
    return np.ascontiguousarray(np.concatenate(outs, axis=0).astype(np.float32))
```

```python
import contextlib
import numpy as np
import concourse.bass as bass
import concourse.mybir as mybir
from concourse.bass_utils import run_bass_kernel_spmd

F32 = mybir.dt.float32
BF16 = mybir.dt.bfloat16
AF = mybir.ActivationFunctionType
ALU = mybir.AluOpType
AX = mybir.AxisListType

NCORES = 8
NB = 4
D = 1024
S = 2048
L = 256
T = L + S
NT = T // 128
BLKS = [(0, 256), (256, 768), (768, 1280), (1280, 1792), (1792, 2304)]
EPS = 1e-6
N_IN = 2336
NEXP = 16
GLA_CUT = 0


def _dsize(dt):
    return 4 if dt == F32 else 2


def _auto_keys(name, ap, G):
    s = _dsize(ap.dtype)
    dims = ap.ap
    pstep = dims[0][0]
    base = ap.offset % pstep if pstep > 0 else ap.offset
    free = dims[1:]
    offs = [0]
    for (st, n) in free[:-1]:
        if st == 0:
            continue
        offs = [o + st * i for o in offs for i in range(n)]
    lst, ln = free[-1]
    span = abs(lst) * (ln - 1) + 1
    keys = set()
    for o in offs:
        b0 = (base + o) * s
        b1 = (base + o + span) * s - 1
        for g in range(b0 // G, b1 // G + 1):
            keys.add((name, g))
    return tuple(keys)


class V:
    def __init__(self, ap, keys, excl=False):
        self.ap = ap
        self.keys = tuple(keys)
        self.excl = excl

    def __getitem__(self, idx):
        return V(self.ap[idx], self.keys, self.excl)

    def re(self, pat, **kw):
        return V(self.ap.rearrange(pat, **kw), self.keys, self.excl)

    def bc(self, axis, shape):
        return V(self.ap.unsqueeze(axis).to_broadcast(list(shape)), self.keys, self.excl)

    def bcast(self, shape):
        return V(self.ap.to_broadcast(list(shape)), self.keys, self.excl)


class T_:
    def __init__(self, ap, name, G, excl=False):
        self.ap = ap
        self.name = name
        self.G = G
        self.excl = excl

    def __getitem__(self, idx):
        a = self.ap[idx]
        return V(a, _auto_keys(self.name, a, self.G), self.excl)

    def all(self):
        return V(self.ap, _auto_keys(self.name, self.ap, self.G), self.excl)


def DR(ap):
    return V(ap, ())


class Prog:
    ENGS = ("pe", "act", "dve", "pool", "sp")

    def __init__(self, nc):
        self.nc = nc
        self.es = contextlib.ExitStack()
        self.ops = []
        self.last_w = {}
        self.readers = {}
        self.tag_cum = {}
        self.tag_sem = {}
        self.eng_sem = {}
        for e in self.ENGS:
            self.eng_sem[e] = self.es.enter_context(nc.semaphore("s_" + e))

    def sbuf(self, name, shape, dtype, G=1024):
        t = self.es.enter_context(self.nc.sbuf_tensor(name, list(shape), dtype))
        return T_(t[:], name, G)

    def psum(self, name, shape, dtype=F32, G=2048):
        t = self.es.enter_context(self.nc.psum_tensor(name, list(shape), dtype))
        return T_(t[:], name, G, excl=True)

    def _tag(self, tag):
        if tag not in self.tag_sem:
            self.tag_sem[tag] = self.es.enter_context(self.nc.semaphore("d_" + str(tag)))
            self.tag_cum[tag] = 0
        return self.tag_sem[tag]

    def op(self, eng, fn, reads=(), writes=(), dma_tag=None):
        idx = len(self.ops)
        rk = [k for v in reads if not v.excl for k in v.keys]
        wk = [k for v in writes for k in v.keys] + [k for v in reads if v.excl for k in v.keys]
        deps = set()
        for k in rk + wk:
            w = self.last_w.get(k)
            if w is not None:
                deps.add(w)
        for k in wk:
            r = self.readers.get(k)
            if r:
                deps.update(r.values())
        waits = []
        for d in deps:
            Dd = self.ops[d]
            if Dd["dma_tag"] is not None:
                waits.append(("dma", Dd["dma_tag"], self.tag_cum[Dd["dma_tag"]]))
            else:
                if Dd["eng"] == eng and eng == "pe":
                    continue
                Dd["flag"] = True
                waits.append(("eng", Dd["eng"], d))
        if dma_tag is not None:
            self._tag(dma_tag)
            self.tag_cum[dma_tag] += 16
        self.ops.append(dict(eng=eng, fn=fn, waits=waits, dma_tag=dma_tag, flag=False, count=None))
        wks = set(wk)
        for k in wks:
            self.last_w[k] = idx
            self.readers[k] = {}
        for k in rk:
            if k not in wks:
                rkey = eng if dma_tag is None else ("dma", idx)
                self.readers.setdefault(k, {})[rkey] = idx
        return idx

    def wait_all_dma(self, eng, tag):
        self.ops.append(dict(eng=eng, fn=None, waits=[("dma", tag, self.tag_cum[tag])], dma_tag=None,
                             flag=False, count=None))

    def dma(self, eng, out, in_, tag):
        return self.op(eng, lambda e: e.dma_start(out=out.ap, in_=in_.ap), reads=[in_], writes=[out], dma_tag=tag)

    def mm(self, out, lhsT, rhs, start=True, stop=True):
        return self.op("pe", lambda e: e.matmul(out.ap, lhsT.ap, rhs.ap, start=start, stop=stop),
                       reads=[lhsT, rhs], writes=[out])

    def transpose(self, out, in_, ident):
        return self.op("pe", lambda e: e.transpose(out.ap, in_.ap, ident.ap), reads=[in_, ident], writes=[out])

    def act(self, out, in_, func, bias=None, scale=None):
        reads = [in_]
        kw = {}
        if bias is not None:
            if isinstance(bias, V):
                reads.append(bias)
                kw["bias"] = bias.ap
            else:
                kw["bias"] = bias
        if scale is not None:
            if isinstance(scale, V):
                reads.append(scale)
                kw["scale"] = scale.ap
            else:
                kw["scale"] = scale
        return self.op("act", lambda e: e.activation(out.ap, in_.ap, func, **kw), reads=reads, writes=[out])

    def tt(self, out, a, b, op, eng="dve"):
        return self.op(eng, lambda e: e.tensor_tensor(out.ap, a.ap, b.ap, op), reads=[a, b], writes=[out])

    def ts(self, out, a, s1, op0, s2=None, op1=None, eng="dve"):
        reads = [a]
        x1 = s1
        if isinstance(s1, V):
            reads.append(s1)
            x1 = s1.ap
        x2 = s2
        if isinstance(s2, V):
            reads.append(s2)
            x2 = s2.ap
        if op1 is None:
            return self.op(eng, lambda e: e.tensor_scalar(out.ap, a.ap, x1, None, op0), reads=reads, writes=[out])
        return self.op(eng, lambda e: e.tensor_scalar(out.ap, a.ap, x1, x2, op0, op1), reads=reads, writes=[out])

    def stt(self, out, a, s, b, op0, op1, eng="dve"):
        reads = [a, b]
        x = s
        if isinstance(s, V):
            reads.append(s)
            x = s.ap
        return self.op(eng, lambda e: e.scalar_tensor_tensor(out.ap, a.ap, x, b.ap, op0, op1),
                       reads=reads, writes=[out])

    def copy(self, out, in_, eng="dve"):
        if eng == "act":
            return self.op(eng, lambda e: e.copy(out.ap, in_.ap), reads=[in_], writes=[out])
        return self.op(eng, lambda e: e.tensor_copy(out.ap, in_.ap), reads=[in_], writes=[out])

    def memset(self, out, val, eng="dve"):
        return self.op(eng, lambda e: e.memset(out.ap, val), reads=[], writes=[out])

    def reduce(self, out, in_, op, eng="dve"):
        return self.op(eng, lambda e: e.tensor_reduce(out.ap, in_.ap, AX.X, op), reads=[in_], writes=[out])

    def recip(self, out, in_):
        return self.op("dve", lambda e: e.reciprocal(out.ap, in_.ap), reads=[in_], writes=[out])

    def emit(self):
        nc = self.nc
        cnt = {e: 0 for e in self.ENGS}
        for o in self.ops:
            if o["dma_tag"] is None and o["flag"]:
                cnt[o["eng"]] += 1
                o["count"] = cnt[o["eng"]]
        engobj = {"pe": "tensor", "act": "scalar", "dve": "vector", "pool": "gpsimd", "sp": "sync"}
        stats = {e: [0, 0] for e in self.ENGS}
        with nc.Block() as block:
            for e in self.ENGS:
                mine = [o for o in self.ops if o["eng"] == e]

                def body(eng, mine=mine, e=e):
                    waited = {}
                    for o in mine:
                        need = {}
                        for w in o["waits"]:
                            if w[0] == "dma":
                                sem = self.tag_sem[w[1]]
                                val = w[2]
                                key = ("d", w[1])
                            else:
                                sem = self.eng_sem[w[1]]
                                val = self.ops[w[2]]["count"]
                                key = ("e", w[1])
                            if key not in need or need[key][1] < val:
                                need[key] = (sem, val)
                        for key, (sem, val) in need.items():
                            if waited.get(key, 0) < val:
                                eng.wait_ge(sem, val)
                                waited[key] = val
                                stats[e][1] += 1
                        if o["fn"] is None:
                            continue
                        ins = o["fn"](eng)
                        stats[e][0] += 1
                        if o["dma_tag"] is not None:
                            ins.then_inc(self.tag_sem[o["dma_tag"]], 16)
                        elif o["flag"]:
                            ins.then_inc(self.eng_sem[e], 1)

                getattr(block, engobj[e])(body)
        return stats

    def close(self):
        self.es.close()


class Ring:
    def __init__(self, items):
        self.items = items
        self.i = 0

    def next(self):
        x = self.items[self.i % len(self.items)]
        self.i += 1
        return x


class Arena:
    def __init__(self, P, name, nbytes, G=1024):
        self.t = P.es.enter_context(P.nc.sbuf_tensor(name, [128, nbytes // 2], BF16))
        self.name = name
        self.G = G
        self.nbytes = nbytes

    def view(self, off, shape, dtype, parts=128):
        n = 1
        for s in shape:
            n *= s
        nb = n * _dsize(dtype)
        assert off % 4 == 0 and off + nb <= self.nbytes, (off, nb, self.nbytes)
        ap = self.t[0:parts, off // 2:(off + nb) // 2]
        if dtype == F32:
            ap = ap.bitcast(F32)
        if len(shape) == 2:
            ap = ap.rearrange("p (a b) -> p a b", a=shape[0])
        elif len(shape) == 3:
            ap = ap.rearrange("p (a b c) -> p a b c", a=shape[0], b=shape[1])
        elif len(shape) == 4:
            ap = ap.rearrange("p (a b c d) -> p a b c d", a=shape[0], b=shape[1], c=shape[2])
        return T_(ap, self.name, self.G)


def build_program(nb=NB, nlayers=2, dbg_stage=None):
    nc = bass.Bass("TRN2", target_bir_lowering=False)
    P = Prog(nc)

    def din(name, shape, dt=F32):
        return nc.dram_tensor(name, list(shape), dt, kind="ExternalInput").ap()

    xT_d = din("xT", [nb, 128, 8, T])
    cT_d = din("cT", [128, 8, 5])
    wada_d = din("w_ada_r", [2, 48, 128, 8, 128])
    bada_d = din("b_adaT", [128, 2, 48])
    gnorm_d = din("g_normT", [128, 2, 2, 8])
    wfm_d = din("w_fm", [2, 17, 128, 8, 128])
    wtm_d = din("w_tm", [2, 128, 8, 768])
    qkg_d = din("qkg", [128, 2, 2])
    cos_d = din("cosT", [128, T])
    sin_d = din("sinT", [128, T])
    sink_d = din("sink", [128, 16])
    gw_d = din("gla_gw", [128, 2, 256])
    gng_d = din("gla_ng", [128, 2, 64])
    cw_d = din("conv_wT", [128, 2, 2, 3])
    wout_d = din("w_out_r", [2, 128, 8, 1024])
    wr_d = din("w_router_r", [128, 8, 16])
    br_d = din("b_router_r", [128, 16])
    wg_d = din("wg_r", [2, NEXP, 128, 8, 512])
    wu_d = din("wu_r", [2, NEXP, 128, 8, 512])
    wd_d = din("wd_r", [2, NEXP, 128, 4, 1024])
    cst_d = din("consts", [128, 12, 128])
    bdm_d = din("bdmask", [128, 256])
    sel_d = din("sel", [128, 16, 128])
    out_d = nc.dram_tensor("outT", [nb, 128, 8, S], F32, kind="ExternalOutput").ap()

    XT = P.sbuf("XT", [128, 8, T], F32, G=1024)
    HT = P.sbuf("HT", [128, 8, T], BF16, G=512)
    U = Arena(P, "U", 65536, G=256)
    U2 = Arena(P, "U2", 6144, G=256)
    IDENT = P.sbuf("IDENT", [128, 128], F32)
    CB16 = P.sbuf("CB16", [128, 8, 128], BF16, G=256)
    (C_ONES, C_LINC, C_USTR, C_UINC, C_LSTR, C_PERM, C_BD64, C_IDB) = range(8)
    HMASK = P.sbuf("HMASK", [128, 8], F32)
    BDM = P.sbuf("BDM", [128, 256], F32)
    MOD = P.sbuf("MOD", [128, 2, 48, 5], F32, G=64)
    GS = P.sbuf("GS", [128, 2, 2, 8, 5], F32, G=64)
    GN = P.sbuf("GN", [128, 2, 2, 8], F32, G=64)
    BADA = P.sbuf("BADA", [128, 2, 48], F32, G=64)
    CT = P.sbuf("CT", [128, 8, 5], F32)
    QKG = P.sbuf("QKG", [128, 2, 2], F32)
    CW = P.sbuf("CW", [128, 2, 2, 3], F32)
    GNG = P.sbuf("GNG", [128, 2, 64], F32)
    WR32 = P.sbuf("WR32", [128, 8, 16], F32)
    BRT = P.sbuf("BRT", [128, 16], F32, G=64)
    SKE = P.sbuf("SKE", [128, 16], F32, G=64)
    GWB = P.sbuf("GWB", [128, 2, 256], BF16)
    GP = P.sbuf("GP", [128, 4, 128], F32, G=2048)
    SQ = [P.sbuf("SQ%d" % i, [128, 512], BF16) for i in range(2)]
    RS = P.sbuf("RS", [128, 512], F32, G=2048)
    TMPF = [P.sbuf("TMPF%d" % i, [128, 512], F32, G=2048) for i in range(2)]
    HF = [U.view(45568 + i * 2048, [512], F32) for i in range(2)]
    SMALL = P.sbuf("SMALL", [128, 16, 64], F32, G=256)
    PS = [P.psum("PS%d" % i, [128, 512], F32) for i in range(7)]
    PT = P.psum("PT", [128, 1024], BF16)

    sqr = Ring(SQ)
    tmpr = Ring(TMPF)
    hfr = Ring(HF)

    def cst(i):
        return CB16[:, i, :]

    P.dma("sp", IDENT.all(), DR(cst_d[:, 11, :]), "c0")
    P.dma("pool", CB16[:, 0:8, :], DR(cst_d[:, 0:8, :]), "c1")
    P.dma("sp", HMASK.all(), DR(cst_d[:, 8, 0:8]), "c0")
    P.dma("sp", BDM.all(), DR(bdm_d), "c0")
    P.dma("sp", BADA.all(), DR(bada_d), "c0")
    P.dma("sp", GN.all(), DR(gnorm_d), "c0")
    P.dma("sp", CT.all(), DR(cT_d), "c0")
    P.dma("sp", QKG.all(), DR(qkg_d), "c0")
    P.dma("sp", CW.all(), DR(cw_d), "c0")
    P.dma("sp", GNG.all(), DR(gng_d), "c0")
    P.dma("sp", WR32.all(), DR(wr_d), "c0")
    P.dma("sp", BRT.all(), DR(br_d), "c0")
    P.dma("sp", SKE.all(), DR(sink_d), "c0")
    P.dma("pool", GWB.all(), DR(gw_d), "c1")
    P.memset(GP.all(), 0.0)
    P.act(SKE.all(), SKE.all(), AF.Exp)

    SC = P.sbuf("SC", [128, 8, 5], F32)
    P.act(SC.all(), CT.all(), AF.Exp, scale=-1.0)
    P.ts(SC.all(), SC.all(), 1.0, ALU.add)
    P.recip(SC.all(), SC.all())
    P.tt(SC.all(), SC.all(), CT.all(), ALU.mult)
    WA = [U.view(i * 4096, [8, 128], F32) for i in range(3)]
    war = Ring(WA)
    wa_tags = Ring(["wa0", "wa1", "wa2"])
    for l in range(nlayers):
        slots = {}

        def ld(n):
            w = war.next()
            P.dma("sp", w.all(), DR(wada_d[l, n]), wa_tags.next())
            slots[n] = w

        ld(0)
        ld(1)
        for n in range(48):
            if n + 2 < 48:
                ld(n + 2)
            w = slots.pop(n)
            for k in range(8):
                P.mm(PS[0][:, 0:5], w[:, k, :], SC[:, k, :], start=(k == 0), stop=(k == 7))
            P.act(MOD[:, l, n, :], PS[0][:, 0:5], AF.Identity, bias=BADA[:, l, n:n + 1], scale=1.0)
        for s in range(2):
            for c in range(8):
                P.ts(GS[:, l, s, c, :], MOD[:, l, (3 * s + 1) * 8 + c, :], 1.0, ALU.add,
                     GN[:, l, s, c:c + 1], ALU.mult)

    RBANK = [PS[1], PS[4], PS[5], PS[6]]

    def modv(l, kind, c, row):
        return MOD[:, l, kind * 8 + c, row:row + 1]

    def norm_phase(l, s, j, blks, router=None):
        for bi in blks:
            t0, t1 = BLKS[bi]
            n = t1 - t0
            row = 4 if bi == 0 else j
            for c in range(8):
                q = sqr.next()
                P.act(q[:, 0:n], XT[:, c, t0:t1], AF.Square)
                P.mm(PS[0][:, 0:n], cst(C_ONES), q[:, 0:n], start=(c == 0), stop=(c == 7))
            P.act(RS[:, 0:n], PS[0][:, 0:n], AF.Ln, bias=EPS, scale=1.0 / D)
            P.act(RS[:, 0:n], RS[:, 0:n], AF.Exp, scale=-0.5)
            for c in range(8):
                tm = tmpr.next()
                P.stt(tm[:, 0:n], XT[:, c, t0:t1], GS[:, l, s, c, row:row + 1], RS[:, 0:n], ALU.mult, ALU.mult)
                P.act(HT[:, c, t0:t1], tm[:, 0:n], AF.Identity, bias=modv(l, 3 * s, c, row), scale=1.0)
                if router is not None:
                    hf = hfr.next()
                    P.ts(hf[:, 0:n], tm[:, 0:n], modv(l, 3 * s, c, row), ALU.add)
                    for q4 in range(n // 128):
                        P.mm(RBANK[q4][:, 0:16], hf[:, q4 * 128:(q4 + 1) * 128], WR32[:, c, :],
                             start=(c == 0), stop=(c == 7))
            if router is not None:
                router(bi, n // 128)

    wfm_slots = [U.view(57344 + i * 2048, [8, 128], BF16) for i in range(2)]
    wfm_ring = Ring(wfm_slots)
    wfm_tags = Ring(["wf0", "wf1"])

    def load_wfm(l, ch):
        w = wfm_ring.next()
        P.dma("pool", w.all(), DR(wfm_d[l, ch]), wfm_tags.next())
        return w

    projps = Ring([PS[2], PS[3]])

    def proj_fm(w, bi):
        t0, t1 = BLKS[bi]
        ps = projps.next()
        for k in range(8):
            P.mm(ps[:, 0:t1 - t0], w[:, k, :], HT[:, k, t0:t1], start=(k == 0), stop=(k == 7))
        return ps

    WO = U.view(53248, [2, 1024], BF16)

    def wout_partial(l, j, kc0, YT, blks):
        P.dma("pool", WO.all(), DR(wout_d[l, :, kc0:kc0 + 2, :]), "wo")
        for bi in blks:
            t0, t1 = BLKS[bi]
            n = t1 - t0
            row = 4 if bi == 0 else j
            for d in range(8):
                ps = projps.next()
                for k in range(2):
                    P.mm(ps[:, 0:n], WO[:, k, d * 128:(d + 1) * 128], YT[:, k, t0:t1], start=(k == 0), stop=(k == 1))
                P.stt(XT[:, d, t0:t1], ps[:, 0:n], modv(l, 2, d, row), XT[:, d, t0:t1], ALU.mult, ALU.add)

    def conv_group(l, j, blks_out):
        CBf = U.view(0, [T], F32)
        CCf = U.view(9216, [T], F32)
        UU = U.view(18432, [T], F32)
        ACC = U.view(27648, [T], F32)
        YT = U.view(36864, [2, T], BF16)
        for cc in range(2):
            for which, dst in ((9 + cc, CBf), (11 + cc, CCf), (13 + cc, None)):
                w = load_wfm(l, which)
                for bi in range(5):
                    t0, t1 = BLKS[bi]
                    ps = proj_fm(w, bi)
                    if dst is not None:
                        P.copy(dst[:, t0:t1], ps[:, 0:t1 - t0], eng="act")
                    else:
                        P.tt(UU[:, t0:t1], ps[:, 0:t1 - t0], CCf[:, t0:t1], ALU.mult)
            P.ts(ACC.all(), UU.all(), CW[:, l, cc, 1:2], ALU.mult)
            for (a, b) in ((0, L), (L, T)):
                P.stt(ACC[:, a + 1:b], UU[:, a:b - 1], CW[:, l, cc, 0:1], ACC[:, a + 1:b], ALU.mult, ALU.add)
                P.stt(ACC[:, a:b - 1], UU[:, a + 1:b], CW[:, l, cc, 2:3], ACC[:, a:b - 1], ALU.mult, ALU.add)
            P.tt(YT[:, cc, :], CBf.all(), ACC.all(), ALU.mult)
        wout_partial(l, j, 6, YT, blks_out)

    def attn_group(l, j, kv, with_ctx_out):
        QT = U.view(0, [2, T], BF16)
        KM = U.view(9216, [2, T], BF16)
        VX = U.view(18432, [NT, 128], BF16)
        YT = U.view(23040, [2, T], BF16)
        COS = U.view(32256, [T], BF16)
        SIN = U.view(36864, [T], BF16)
        KN = U.view(41472, [512], BF16)
        QN = U.view(42496, [512], BF16)
        SQB = U.view(43520, [512], BF16)
        EX = [U.view(44544 + i * 1024, [512], BF16) for i in range(3)]
        RD = U.view(47616, [512], F32)
        WV = U.view(49664, [8, 64], BF16)
        RQ = U.view(50688, [512], F32)
        ESK = U.view(61440, [512], F32)
        DEN = U.view(63488, [512], F32)
        exr = Ring(EX)
        sqbr = Ring([SQB, U2.view(0, [512], BF16)])
        qnr = Ring([QN, U2.view(1024, [512], BF16)])
        knr = Ring([KN, U2.view(2048, [512], BF16)])
        rqr = Ring([RQ, U2.view(3072, [512], F32)])
        denr = Ring([DEN, U2.view(0, [512], F32)])
        rdr = Ring([RD, U2.view(2048, [512], F32)])
        ssps = Ring([PS[4], PS[0]])
        rotps = Ring([PS[5], PS[1]])
        P.dma("pool", COS.all(), DR(cos_d), "tab")
        P.dma("pool", SIN.all(), DR(sin_d), "tab")
        for a in range(2):
            for b in range(2):
                idx = 8 * l + 4 * kv + 2 * b + a
                P.copy(ESK[:, (2 * a + b) * 128:(2 * a + b + 1) * 128], SKE[:, idx:idx + 1].bcast([128, 128]), eng="act")
        for which in (2 * kv, 2 * kv + 1, 4 + kv):
            isk = which >= 4
            w = load_wfm(l, which)
            for bi in range(5):
                t0, t1 = BLKS[bi]
                n = t1 - t0
                ps = proj_fm(w, bi)
                sqb = sqbr.next()
                rq = rqr.next()
                qn = qnr.next()
                ssp = ssps.next()
                rtp = rotps.next()
                P.act(sqb[:, 0:n], ps[:, 0:n], AF.Square)
                P.mm(ssp[:, 0:n], cst(C_BD64), sqb[:, 0:n])
                P.act(rq[:, 0:n], ssp[:, 0:n], AF.Ln, bias=EPS, scale=1.0 / 64)
                P.act(rq[:, 0:n], rq[:, 0:n], AF.Exp, scale=-0.5)
                P.stt(qn[:, 0:n], ps[:, 0:n], QKG[:, l, (1 if isk else 0):(2 if isk else 1)], rq[:, 0:n],
                      ALU.mult, ALU.mult)
                P.mm(rtp[:, 0:n], cst(C_PERM), qn[:, 0:n])
                t1_ = tmpr.next()
                t2_ = tmpr.next()
                P.tt(t1_[:, 0:n], qn[:, 0:n], COS[:, t0:t1], ALU.mult)
                P.tt(t2_[:, 0:n], rtp[:, 0:n], SIN[:, t0:t1], ALU.mult)
                if not isk:
                    P.tt(QT[:, which - 2 * kv, t0:t1], t1_[:, 0:n], t2_[:, 0:n], ALU.add)
                else:
                    kn = knr.next()
                    P.tt(kn[:, 0:n], t1_[:, 0:n], t2_[:, 0:n], ALU.add)
                    P.ts(KM[:, 0, t0:t1], kn[:, 0:n], HMASK[:, 0:1], ALU.mult)
                    P.ts(KM[:, 1, t0:t1], kn[:, 0:n], HMASK[:, 1:2], ALU.mult)
        VT = U.view(23040, [T], BF16)
        wv_ = load_wfm(l, 15 + kv)
        for bi in range(5):
            t0, t1 = BLKS[bi]
            ps = proj_fm(wv_, bi)
            P.copy(VT[:, t0:t1], ps[:, 0:t1 - t0], eng="act")
        for i0 in range(0, NT, 8):
            nt_ = min(8, NT - i0)
            for q in range(nt_):
                i = i0 + q
                P.transpose(PT[:, q * 128:(q + 1) * 128], VT[:, i * 128:(i + 1) * 128], cst(C_IDB))
            P.copy(VX[:, i0:i0 + nt_, :], PT[:, 0:nt_ * 128].re("p (q c) -> p q c", q=nt_), eng="act")
        stps = Ring([PS[0], PS[1]])
        numr = Ring([PS[2], PS[3]])
        dnr = Ring([PS[4], PS[5]])
        qtiles = list(range(2, NT)) + ([0, 1] if with_ctx_out else [])
        its = []
        for i in qtiles:
            if i >= 2:
                keys = [(0, None), (1, None)]
                if i - 1 >= 2:
                    keys.append((i - 1, C_UINC))
                keys.append((i, None))
                if i + 1 < NT:
                    keys.append((i + 1, C_LINC))
            else:
                keys = [(0, None), (1, None)]
            for ki, (kt, msk) in enumerate(keys):
                its.append((i, kt, msk, ki == 0, ki == len(keys) - 1))

        def scores(it):
            i, kt, msk, first, lastk = it
            st = stps.next()
            for par in range(2):
                P.mm(st[:, par * 256:(par + 1) * 256].re("p (c q) -> p c q", c=2),
                     KM[:, par, kt * 128:(kt + 1) * 128], QT[:, :, i * 128:(i + 1) * 128])
            return st

        cur = {}
        st_next = scores(its[0])
        for n_, it in enumerate(its):
            i, kt, msk, first, lastk = it
            st = st_next
            if n_ + 1 < len(its):
                st_next = scores(its[n_ + 1])
            if first:
                cur["num"] = numr.next()
                cur["den"] = dnr.next()
            ex = exr.next()
            P.act(ex.all(), st.all(), AF.Exp, scale=0.125)
            if msk is not None:
                P.tt(ex.all().re("p (h q) -> p h q", h=4), ex.all().re("p (h q) -> p h q", h=4),
                     cst(msk).bc(1, [128, 4, 128]), ALU.mult)
            P.mm(cur["num"].all(), VX[:, kt, :], ex.all(), start=first, stop=lastk)
            P.mm(cur["den"].all(), cst(C_ONES), ex.all(), start=first, stop=lastk)
            if lastk:
                den = denr.next()
                rd = rdr.next()
                P.tt(den.all(), cur["den"].all(), ESK.all(), ALU.add)
                P.act(rd.all(), den.all(), AF.Ln)
                P.act(rd.all(), rd.all(), AF.Exp, scale=-1.0)
                for par in range(2):
                    for cc in range(2):
                        cb = (2 * par + cc) * 128
                        P.tt(YT[par * 64:(par + 1) * 64, cc, i * 128:(i + 1) * 128],
                             cur["num"][par * 64:(par + 1) * 64, cb:cb + 128],
                             rd[par * 64:(par + 1) * 64, cb:cb + 128], ALU.mult)
        wout_partial(l, j, 2 * kv, YT, [0, 1, 2, 3, 4] if with_ctx_out else [1, 2, 3, 4])

    def gla_group(l, j, with_ctx_out):
        GQ = U.view(0, [T], BF16)
        GK = U.view(4608, [T], BF16)
        GLW = U.view(9216, [T], BF16)
        KTOK = U.view(13824, [NT, 128], BF16)
        VTOK = U.view(18432, [NT, 256], BF16)
        OF = U.view(27648, [NT, 256], BF16)
        YT = U.view(36864, [2, T], BF16)
        WRG = U.view(46080, [8, 256], BF16)
        WKV = U.view(50176, [8, 384], BF16)
        GALL = U.view(50176, [NT, 256], BF16)
        yb = U2.view(5632, [256], BF16)
        EB, ENB, ESUF = gla_f32[0], gla_f32[1], gla_f32[2]
        SETS = {
            (0, 0): dict(QB=U.view(61440, [128], BF16), KD=U.view(61696, [128], BF16), KB4=U.view(61952, [4, 128], BF16)),
            (0, 1): dict(QB=U2.view(0, [128], BF16), KD=U2.view(256, [128], BF16), KB4=U2.view(512, [4, 128], BF16)),
            (1, 0): dict(QB=U2.view(1536, [128], BF16), KD=U2.view(1792, [128], BF16), KB4=U2.view(2048, [4, 128], BF16)),
            (1, 1): dict(QB=U2.view(3072, [128], BF16), KD=U2.view(3328, [128], BF16), KB4=U2.view(3584, [4, 128], BF16)),
        }
        ATMS = [U.view(62976, [4, 128], BF16), U2.view(4608, [4, 128], BF16)]
        SBF = [U.view(64256 + d * 512, [256], BF16) for d in range(2)]
        S32 = [gla_s32[0], gla_s32[1]]
        KVMS = [gla_s32[2], gla_s32[3]]
        ATT_PS = [PS[0], PS[1]]
        bps = Ring([PS[4], PS[5]])

        P.dma("pool", WKV.all(), DR(wtm_d[l, :, :, 128:512]), "wkv")
        P.dma("pool", WRG.all(), DR(wtm_d[l, :, :, 512:768]), "wrg")
        for which, dst in ((6, GQ), (7, GK), (8, GLW)):
            w = load_wfm(l, which)
            for bi in range(5):
                t0, t1 = BLKS[bi]
                ps = proj_fm(w, bi)
                if which == 6:
                    P.act(dst[:, t0:t1], ps[:, 0:t1 - t0], AF.Identity, scale=32.0 ** -0.5, bias=0.0)
                else:
                    P.copy(dst[:, t0:t1], ps[:, 0:t1 - t0], eng="act")
        P.ts(GLW.all(), GLW.all(), HMASK[:, 6:7], ALU.add)
        for i0 in range(0, NT, 8):
            nt_ = min(8, NT - i0)
            for q in range(nt_):
                i = i0 + q
                P.transpose(PT[:, q * 128:(q + 1) * 128], GK[:, i * 128:(i + 1) * 128], cst(C_IDB))
            P.copy(KTOK[:, i0:i0 + nt_, :], PT[:, 0:nt_ * 128].re("p (q c) -> p q c", q=nt_), eng="act")
        kvr = Ring([PS[0], PS[1], PS[2], PS[3]])
        for i in range(NT):
            pv = kvr.next()
            for k in range(8):
                P.mm(pv[:, 0:256], HT[:, k, i * 128:(i + 1) * 128], WKV[:, k, 128:384], start=(k == 0), stop=(k == 7))
            P.copy(VTOK[:, i, :], pv[:, 0:256], eng="dve")
        zr = Ring([PS[5], PS[4]])
        for i in range(NT):
            zp = zr.next()
            P.mm(zp[:, 0:256], GLW[:, i * 128:(i + 1) * 128], GWB[:, l, :])
            e1 = tmpr.next()
            P.act(e1[:, 0:256], zp[:, 0:256], AF.Exp, scale=-1.0)
            P.act(e1[:, 0:256], e1[:, 0:256], AF.Ln, bias=1.0, scale=1.0)
            P.ts(GALL[:, i, :], e1[:, 0:256], -1.0 / 16.0, ALU.mult)

        order = [list(range(NT)), [1, 0] + list(range(NT - 1, 1, -1))]
        stepof = [{i: n for n, i in enumerate(order[d])} for d in range(2)]

        def stage_b(n, d):
            i = order[d][n]
            need_out = with_ctx_out or i >= 2
            X = SETS[(n % 2, d)]
            Mcum, Msuf = (C_LINC, C_USTR) if d == 0 else (C_UINC, C_LSTR)
            tcol = 127 if d == 0 else 0
            sl = slice(i * 128, (i + 1) * 128)
            g = GALL[:, i, d * 128:(d + 1) * 128]
            bp = bps.next()
            P.mm(bp[:, 0:128], g, cst(Mcum))
            P.mm(bp[:, 128:256], cst(Msuf), g)
            P.act(EB.all(), bp[:, 0:128], AF.Exp)
            P.act(ESUF.all(), bp[:, 128:256], AF.Exp)
            if need_out:
                P.act(ENB.all(), bp[:, 0:128], AF.Exp, scale=-1.0)
            slot = 2 * (n % 2) + d
            P.copy(SMALL[:, 15, slot:slot + 1], EB[:, tcol:tcol + 1], eng="act")
            P.tt(X["KD"].all(), KTOK[:, i, :], ESUF.all(), ALU.mult)
            if need_out:
                P.tt(X["QB"].all(), GQ[:, sl], EB.all(), ALU.mult)
                P.tt(ENB.all(), GK[:, sl], ENB.all(), ALU.mult)
                for h in range(4):
                    P.act(X["KB4"][:, h, :], ENB.all(), AF.Identity, scale=HMASK[:, 2 + h:3 + h], bias=0.0)

        def stage_c(n):
            tiles = [order[d][n] for d in range(2)]
            needs = [with_ctx_out or tiles[d] >= 2 for d in range(2)]
            finals = [stepof[1 - d][tiles[d]] < n for d in range(2)]
            Xs = [SETS[(n % 2, d)] for d in range(2)]
            for d in range(2):
                if needs[d]:
                    for h in range(4):
                        P.mm(ATT_PS[d][:, h * 128:(h + 1) * 128], Xs[d]["KB4"][:, h, :], Xs[d]["QB"].all())
            for d in range(2):
                P.mm(PS[2][:, 0:256] if d == 0 else PS[3][:, 0:256], Xs[d]["KD"].all(), VTOK[:, tiles[d], :])
            for d in range(2):
                if needs[d]:
                    Mmask = C_LINC if d == 0 else C_USTR
                    P.tt(ATMS[d].all(), ATT_PS[d].all().re("p (h q) -> p h q", h=4),
                         cst(Mmask).bc(1, [128, 4, 128]), ALU.mult)
            for d in range(2):
                slot = 2 * (n % 2) + d
                kvp = PS[2] if d == 0 else PS[3]
                P.tt(KVMS[d].all(), kvp[:, 0:256], BDM.all(), ALU.mult)
                P.stt(S32[d].all(), S32[d].all(), SMALL[:, 15, slot:slot + 1], KVMS[d].all(), ALU.mult, ALU.add)
            for d in range(2):
                if not needs[d]:
                    P.copy(SBF[d].all(), S32[d].all(), eng="act")
                    continue
                i = tiles[d]
                sl = slice(i * 128, (i + 1) * 128)
                for h in range(4):
                    hs = slice(h * 64, (h + 1) * 64)
                    P.mm(PS[6][:, hs], Xs[d]["QB"].all(), SBF[d][:, hs], start=True, stop=False)
                    P.mm(PS[6][:, hs], ATMS[d][:, h, :], VTOK[:, i, hs], start=False, stop=True)
                P.copy(SBF[d].all(), S32[d].all(), eng="act")
                if not finals[d]:
                    P.copy(OF[:, i, :], PS[6][:, 0:256], eng="act")
                    continue
                osum = tmpr.next()
                P.tt(osum[:, 0:256], PS[6][:, 0:256], OF[:, i, :], ALU.add)
                for k in range(8):
                    P.mm(PS[6][:, 256:512], HT[:, k, sl], WRG[:, k, :], start=(k == 0), stop=(k == 7))
                sq = tmpr.next()
                P.tt(sq[:, 0:256], osum[:, 0:256], osum[:, 0:256], ALU.mult)
                ssh = SMALL[:, 0, 0:4]
                P.reduce(ssh, sq[:, 0:256].re("p (h e) -> p h e", h=4), ALU.add)
                P.act(ssh, ssh, AF.Ln, bias=EPS, scale=1.0 / 64)
                P.act(ssh, ssh, AF.Exp, scale=-0.5)
                o3 = osum[:, 0:256].re("p (h e) -> p h e", h=4)
                P.tt(o3, o3, ssh.bc(2, [128, 4, 64]), ALU.mult)
                P.tt(o3, o3, GNG[:, l, :].bc(1, [128, 4, 64]), ALU.mult)
                er = sq
                P.act(er[:, 256:512], PS[6][:, 256:512], AF.Exp, scale=-1.0)
                P.act(er[:, 256:512], er[:, 256:512], AF.Ln, bias=1.0, scale=1.0)
                P.act(er[:, 256:512], er[:, 256:512], AF.Exp, scale=-1.0)
                P.tt(er[:, 256:512], er[:, 256:512], PS[6][:, 256:512], ALU.mult)
                P.tt(yb.all(), osum[:, 0:256], er[:, 256:512], ALU.mult)
                for c2 in range(2):
                    P.transpose(PT[:, c2 * 128:(c2 + 1) * 128], yb[:, c2 * 128:(c2 + 1) * 128], cst(C_IDB))
                P.copy(YT[:, :, sl], PT[:, 0:256].re("p (c q) -> p c q", c=2), eng="act")

        for d in range(2):
            P.memset(S32[d].all(), 0.0)
            P.memset(SBF[d].all(), 0.0)
        stage_b(0, 0)
        stage_b(0, 1)
        for n in range(NT):
            if n + 1 < NT:
                stage_b(n + 1, 0)
                stage_b(n + 1, 1)
            stage_c(n)
        wout_partial(l, j, 4, YT, [0, 1, 2, 3, 4] if with_ctx_out else [1, 2, 3, 4])

    gla_f32 = [P.sbuf("GF%d" % i, [128, 128], F32, G=512) for i in range(4)]
    gla_s32 = [P.sbuf("GS32_%d" % i, [128, 256], F32, G=1024) for i in range(4)]

    def moe_phase(l, j, blks):
        GT = U.view(40960, [T], BF16)
        ABUF = [U.view(45568 + i * 4096, [4, 512], BF16) for i in range(2)]
        SG = [U.view(53760 + i * 1024, [512], BF16) for i in range(2)]
        T1 = [U.view(55808 + i * 1024, [512], BF16) for i in range(2)]
        GBC = [U.view(57856 + i * 1024, [512], BF16) for i in range(2)]
        WSL = [U.view(i * 8192, [8, 512], BF16) for i in range(5)]
        WSLD = [U.view(i * 8192, [4, 1024], BF16) for i in range(5)]
        SEL = U.view(59904, [16, 128], BF16)
        P.dma("pool", SEL.all(), DR(sel_d), "sel")
        wring = Ring(list(range(5)))
        wtags = ["we%d" % i for i in range(5)]
        LG = SMALL

        def router(bi, nt):
            t0, _ = BLKS[bi]
            lgb = SMALL[:, 14, 0:nt * 16]
            lg3 = lgb.re("p (q e) -> p q e", q=nt)
            for q4 in range(nt):
                P.tt(lgb[:, q4 * 16:(q4 + 1) * 16], RBANK[q4][:, 0:16], BRT.all(), ALU.add)
            mx = SMALL[:, 0, 0:nt]
            P.reduce(mx, lg3, ALU.max)
            lgs = SMALL[:, 1, 0:nt * 16]
            P.tt(lgs.re("p (q e) -> p q e", q=nt), lg3, mx.bc(2, [128, nt, 16]), ALU.subtract)
            ex = SMALL[:, 2, 0:nt * 16]
            P.act(ex, lgs, AF.Exp)
            ng = nt * 4
            ex4 = ex.re("p (g f) -> p g f", g=ng)
            m1 = SMALL[:, 3, 0:ng]
            P.reduce(m1, ex4, ALU.max)
            eq = SMALL[:, 4, 0:nt * 16]
            eq4 = eq.re("p (g f) -> p g f", g=ng)
            P.tt(eq4, ex4, m1.bc(2, [128, ng, 4]), ALU.is_equal)
            e2 = SMALL[:, 5, 0:nt * 16]
            P.stt(e2, eq, -1.0e30, ex, ALU.mult, ALU.add)
            m2 = SMALL[:, 6, 0:ng]
            P.reduce(m2, e2.re("p (g f) -> p g f", g=ng), ALU.max)
            gsc = SMALL[:, 7, 0:ng]
            P.tt(gsc, m1, m2, ALU.add)
            gm = SMALL[:, 8, 0:nt]
            P.reduce(gm, gsc.re("p (q g) -> p q g", q=nt), ALU.max)
            gsel = SMALL[:, 9, 0:ng]
            P.tt(gsel.re("p (q g) -> p q g", q=nt), gsc.re("p (q g) -> p q g", q=nt), gm.bc(2, [128, nt, 4]),
                 ALU.is_equal)
            selx = SMALL[:, 10, 0:nt * 16]
            selx4 = selx.re("p (g f) -> p g f", g=ng)
            P.tt(selx4, ex4, m2.bc(2, [128, ng, 4]), ALU.is_ge)
            P.tt(selx4, selx4, gsel.bc(2, [128, ng, 4]), ALU.mult)
            wgt = SMALL[:, 11, 0:nt * 16]
            P.tt(wgt, ex, selx, ALU.mult)
            den = SMALL[:, 12, 0:nt]
            P.reduce(den, wgt.re("p (q e) -> p q e", q=nt), ALU.add)
            P.recip(den, den)
            P.tt(GP[:, 0:nt, 0:16], wgt.re("p (q e) -> p q e", q=nt), den.bc(2, [128, nt, 16]), ALU.mult)
            for q4 in range(nt):
                P.transpose(PS[6][:, q4 * 128:(q4 + 1) * 128], GP[:, q4, :], IDENT.all())
            P.copy(GT[:, t0:t0 + nt * 128], PS[6][:, 0:nt * 128], eng="act")

        norm_phase(l, 1, j, blks, router=router)
        if dbg_stage == "gates":
            P.copy(XT[:, 0, :], GT.all(), eng="act")
            return

        gps = Ring([PS[0], PS[1]])
        ups = Ring([PS[2], PS[3]])
        ops_ = Ring([PS[4], PS[5]])
        abr = Ring(ABUF)
        sgr = Ring(SG)
        t1r = Ring(T1)
        gbr = Ring(GBC)

        def load_gu(e):
            sl = [wring.next() for _ in range(3)]
            P.dma("pool", WSL[sl[0]].all(), DR(wg_d[l, e]), wtags[sl[0]])
            P.dma("pool", WSL[sl[1]].all(), DR(wu_d[l, e]), wtags[sl[1]])
            return sl

        def load_d(e, sl):
            P.dma("pool", WSLD[sl[2]].all(), DR(wd_d[l, e]), wtags[sl[2]])

        def gu_steps(e, bi, sl):
            t0, t1 = BLKS[bi]
            n = t1 - t0
            gb = gbr.next()
            ab = abr.next()

            def pre():
                P.mm(PS[6][:, 0:n], SEL[:, e, :], GT[:, t0:t1])
                P.copy(gb[:, 0:n], PS[6][:, 0:n], eng="act")

            def fstep(f):
                g = gps.next()
                u = ups.next()
                for k in range(8):
                    P.mm(g[:, 0:n], WSL[sl[0]][:, k, f * 128:(f + 1) * 128], HT[:, k, t0:t1], start=(k == 0), stop=(k == 7))
                for k in range(8):
                    P.mm(u[:, 0:n], WSL[sl[1]][:, k, f * 128:(f + 1) * 128], HT[:, k, t0:t1], start=(k == 0), stop=(k == 7))
                sg = sgr.next()
                P.act(sg[:, 0:n], g[:, 0:n], AF.Silu)
                t1_ = t1r.next()
                P.tt(t1_[:, 0:n], u[:, 0:n], sg[:, 0:n], ALU.mult)
                P.tt(ab[:, f, 0:n], t1_[:, 0:n], gb[:, 0:n], ALU.mult)

            return pre, fstep, ab

        def down_step(e, bi, sl, ab, d):
            t0, t1 = BLKS[bi]
            n = t1 - t0
            row = 4 if bi == 0 else j
            wd = WSLD[sl[2]]
            o = ops_.next()
            for f in range(4):
                P.mm(o[:, 0:n], wd[:, f, d * 128:(d + 1) * 128], ab[:, f, 0:n], start=(f == 0), stop=(f == 3))
            P.stt(XT[:, d, t0:t1], o[:, 0:n], modv(l, 5, d, row), XT[:, d, t0:t1], ALU.mult, ALU.add)

        pend = None
        nxt = load_gu(0)
        load_d(0, nxt)
        for e in range(NEXP):
            sl = nxt
            for bi in blks:
                pre, fstep, ab = gu_steps(e, bi, sl)
                pre()
                for f in range(4):
                    fstep(f)
                    if pend is not None:
                        down_step(*pend, 2 * f)
                        down_step(*pend, 2 * f + 1)
                pend = (e, bi, sl, ab)
                if bi == blks[0] and e + 1 < NEXP:
                    nxt = load_gu(e + 1)
            if e + 1 < NEXP:
                load_d(e + 1, nxt)
        for d in range(8):
            down_step(*pend, d)

    for j in range(nb):
        for c in range(8):
            P.dma("sp", XT[:, c, :], DR(xT_d[j, :, c, :]), "xin")
        for l in range(nlayers):
            last = (l == nlayers - 1) and nlayers == 2
            allb = [0, 1, 2, 3, 4]
            latb = [1, 2, 3, 4]
            norm_phase(l, 0, j, allb)
            if dbg_stage == "norm0":
                for c in range(8):
                    P.copy(XT[:, c, :], HT[:, c, :], eng="act")
                break
            conv_group(l, j, latb if last else allb)
            if dbg_stage == "conv":
                break
            gla_group(l, j, not last)
            if dbg_stage == "gla":
                break
            for kv in range(2):
                attn_group(l, j, kv, not last)
            if dbg_stage == "mix":
                break
            moe_phase(l, j, latb if last else allb)
            if dbg_stage == "gates":
                break
        for c in range(8):
            P.dma("sp", DR(out_d[j, :, c, :]), XT[:, c, L:T], "xout")
        if dbg_stage == "norm0":
            pass
    P.wait_all_dma("sp", "xout")
    stats = P.emit()
    P.close()
    return nc, stats


def _rope_tables():
    rows = S // 64
    r, c = np.meshgrid(np.arange(rows), np.arange(64), indexing="ij")
    nf = 16
    inv = (10000.0 ** (-np.arange(nf, dtype=np.float32) / nf)).astype(np.float32)
    ang = np.concatenate([r.reshape(-1, 1).astype(np.float32) * inv, c.reshape(-1, 1).astype(np.float32) * inv], -1)
    cos = np.cos(ang).astype(np.float32)
    sin = np.sin(ang).astype(np.float32)
    cosT = np.ones((128, T), np.float32)
    sinT = np.zeros((128, T), np.float32)
    for p in range(128):
        d = p % 64
        i = d % 32
        cosT[p, L:] = cos[:, i]
        sinT[p, L:] = (-sin[:, i]) if d < 32 else sin[:, i]
    return cosT, sinT


def _consts():
    c = np.zeros((128, 12, 128), np.float32)
    s = np.arange(128)[:, None]
    t = np.arange(128)[None, :]
    c[:, 0] = 1.0
    c[:, 1] = (s <= t)
    c[:, 2] = (s > t)
    c[:, 3] = (s >= t)
    c[:, 4] = (s < t)
    perm = np.zeros((128, 128), np.float32)
    for m in range(128):
        partner = m + 32 if (m % 64) < 32 else m - 32
        perm[partner, m] = 1.0
    c[:, 5] = perm
    c[:, 6] = ((s // 64) == (t // 64))
    c[:, 7] = np.eye(128)
    hm = np.zeros((128, 128), np.float32)
    hm[:64, 0] = 1.0
    hm[64:, 1] = 1.0
    for h in range(4):
        hm[32 * h:32 * h + 32, 2 + h] = 1.0
    hm[32, 6] = 1.0
    c[:, 8] = hm
    c[:, 11] = np.eye(128)
    bdm = ((np.arange(128)[:, None] // 32) == (np.arange(256)[None, :] // 64)).astype(np.float32)
    sel = np.zeros((128, 16, 128), np.float32)
    for e in range(16):
        sel[e, e, :] = 1.0
    return c, bdm, sel


def _prep_weights(inp):
    f = lambda a: np.ascontiguousarray(np.asarray(a, dtype=np.float32))
    w = {}
    w_ada = f(inp["w_ada"])
    w["w_ada_r"] = f(w_ada.reshape(2, 8, 128, 48, 128).transpose(0, 3, 2, 1, 4))
    w["b_adaT"] = f(f(inp["b_ada"]).reshape(2, 48, 128).transpose(2, 0, 1))
    gn = np.stack([f(inp["norm_mix_g"]), f(inp["norm_ffn_g"])], 1)
    w["g_normT"] = f(gn.reshape(2, 2, 8, 128).transpose(3, 0, 1, 2))
    w_in = f(inp["w_in"])
    cols = []
    for c in range(4):
        cols.append(np.arange(c * 128, (c + 1) * 128))
    cols.append(np.concatenate([np.arange(512, 576), np.arange(512, 576)]))
    cols.append(np.concatenate([np.arange(576, 640), np.arange(576, 640)]))
    cols.append(np.arange(768, 896))
    cols.append(np.arange(896, 1024))
    cols.append(None)
    for b0 in (1568, 1824, 2080):
        cols.append(np.arange(b0, b0 + 128))
        cols.append(np.arange(b0 + 128, b0 + 256))
    cols.append(np.concatenate([np.arange(640, 704), np.arange(640, 704)]))
    cols.append(np.concatenate([np.arange(704, 768), np.arange(704, 768)]))
    wfm = np.zeros((2, 17, 1024, 128), np.float32)
    for i, cidx in enumerate(cols):
        if cidx is None:
            wfm[:, i, :, 0:32] = w_in[:, :, 1536:1568]
        else:
            wfm[:, i] = w_in[:, :, cidx]
    w["w_fm"] = f(wfm.reshape(2, 17, 8, 128, 128).transpose(0, 1, 3, 2, 4))
    tmc = np.concatenate([np.arange(640, 768), np.arange(896, 1024), np.arange(1024, 1280), np.arange(1280, 1536)])
    w["w_tm"] = f(w_in[:, :, tmc].reshape(2, 8, 128, 768).transpose(0, 2, 1, 3))
    qg = f(inp["q_norm_g"])
    kg = f(inp["k_norm_g"])
    qkg = np.zeros((128, 2, 2), np.float32)
    for p in range(128):
        qkg[p, :, 0] = qg[:, p % 64]
        qkg[p, :, 1] = kg[:, p % 64]
    w["qkg"] = qkg
    w["cosT"], w["sinT"] = _rope_tables()
    w["sink"] = f(np.broadcast_to(f(inp["attn_sink"]).reshape(1, 16), (128, 16)))
    gw = f(inp["gla_gate_w"])
    gwm = np.zeros((128, 2, 256), np.float32)
    gwm[0:16, :, 0:128] = gw[:, 0].transpose(1, 0, 2)
    gwm[16:32, :, 128:256] = gw[:, 1].transpose(1, 0, 2)
    gwm[32] = f(inp["gla_gate_b"]).reshape(2, 256)
    w["gla_gw"] = gwm
    w["gla_ng"] = f(np.broadcast_to(f(inp["gla_norm_g"])[None], (128, 2, 64)))
    cw = f(inp["conv_w"])
    w["conv_wT"] = f(cw.reshape(2, 3, 2, 128).transpose(3, 0, 2, 1))
    w["w_out_r"] = f(f(inp["w_out"]).reshape(2, 8, 128, 1024).transpose(0, 2, 1, 3))
    w["w_router_r"] = f(f(inp["w_router"]).reshape(8, 128, 16).transpose(1, 0, 2))
    w["b_router_r"] = f(np.broadcast_to(f(inp["b_router"]).reshape(1, 16), (128, 16)))
    w["wg_r"] = f(f(inp["w_gate_e"]).reshape(2, 16, 8, 128, 512).transpose(0, 1, 3, 2, 4))
    w["wu_r"] = f(f(inp["w_up_e"]).reshape(2, 16, 8, 128, 512).transpose(0, 1, 3, 2, 4))
    w["wd_r"] = f(f(inp["w_down_e"]).reshape(2, 16, 4, 128, 1024).transpose(0, 1, 3, 2, 4))
    c, bdm, sel = _consts()
    w["consts"] = c
    w["bdmask"] = bdm
    w["sel"] = sel
    return w


def _prep_core(inp, b0, nb):
    x = np.asarray(inp["x"], dtype=np.float32)[b0:b0 + nb]
    ctx = np.asarray(inp["ctx"], dtype=np.float32)[b0:b0 + nb]
    full = np.concatenate([ctx, x], axis=1)
    xT = np.ascontiguousarray(full.reshape(nb, T, 8, 128).transpose(0, 3, 2, 1))
    c5 = np.zeros((5, D), np.float32)
    cc = np.asarray(inp["c"], dtype=np.float32)[b0:b0 + nb]
    c5[:nb] = cc
    c5[4] = np.asarray(inp["c_ctx"], dtype=np.float32)
    cT = np.ascontiguousarray(c5.reshape(5, 8, 128).transpose(2, 1, 0))
    return {"xT": xT, "cT": cT}


_CACHE = {}


def kernel(**inputs):
    w = _prep_weights(inputs)
    if "nc" not in _CACHE:
        _CACHE["nc"] = build_program(NB, 2)[0]
    nc = _CACHE["nc"]
    in_maps = []
    for core in range(NCORES):
        m = dict(w)
        m.update(_prep_core(inputs, core * NB, NB))
        in_maps.append(m)
    res = run_bass_kernel_spmd(nc, in_maps, core_ids=list(range(NCORES)))
    outs = []
    for core in range(NCORES):
        o = np.asarray(res.results[core]["outT"])
        outs.append(o.transpose(0, 3, 2, 1).reshape(NB, S, D))
    return np.ascontiguousarray(np.concatenate(outs, axis=0).astype(np.float32))
```

```python
import contextlib
import numpy as np
import concourse.bass as bass
import concourse.mybir as mybir
from concourse.bass_utils import run_bass_kernel_spmd

F32 = mybir.dt.float32
BF16 = mybir.dt.bfloat16
AF = mybir.ActivationFunctionType
ALU = mybir.AluOpType
AX = mybir.AxisListType

NCORES = 8
NB = 4
D = 1024
S = 2048
L = 256
T = L + S
NT = T // 128
BLKS = [(0, 256), (256, 768), (768, 1280), (1280, 1792), (1792, 2304)]
EPS = 1e-6
N_IN = 2336
NEXP = 16
GLA_CUT = 0


def _dsize(dt):
    return 4 if dt == F32 else 2


def _auto_keys(name, ap, G):
    s = _dsize(ap.dtype)
    dims = ap.ap
    pstep = dims[0][0]
    base = ap.offset % pstep if pstep > 0 else ap.offset
    free = dims[1:]
    offs = [0]
    for (st, n) in free[:-1]:
        if st == 0:
            continue
        offs = [o + st * i for o in offs for i in range(n)]
    lst, ln = free[-1]
    span = abs(lst) * (ln - 1) + 1
    keys = set()
    for o in offs:
        b0 = (base + o) * s
        b1 = (base + o + span) * s - 1
        for g in range(b0 // G, b1 // G + 1):
            keys.add((name, g))
    return tuple(keys)


class V:
    def __init__(self, ap, keys, excl=False):
        self.ap = ap
        self.keys = tuple(keys)
        self.excl = excl

    def __getitem__(self, idx):
        return V(self.ap[idx], self.keys, self.excl)

    def re(self, pat, **kw):
        return V(self.ap.rearrange(pat, **kw), self.keys, self.excl)

    def bc(self, axis, shape):
        return V(self.ap.unsqueeze(axis).to_broadcast(list(shape)), self.keys, self.excl)

    def bcast(self, shape):
        return V(self.ap.to_broadcast(list(shape)), self.keys, self.excl)


class T_:
    def __init__(self, ap, name, G, excl=False):
        self.ap = ap
        self.name = name
        self.G = G
        self.excl = excl

    def __getitem__(self, idx):
        a = self.ap[idx]
        return V(a, _auto_keys(self.name, a, self.G), self.excl)

    def all(self):
        return V(self.ap, _auto_keys(self.name, self.ap, self.G), self.excl)


def DR(ap):
    return V(ap, ())


class Prog:
    ENGS = ("pe", "act", "dve", "pool", "sp")

    def __init__(self, nc):
        self.nc = nc
        self.es = contextlib.ExitStack()
        self.ops = []
        self.last_w = {}
        self.readers = {}
        self.tag_cum = {}
        self.tag_sem = {}
        self.eng_sem = {}
        for e in self.ENGS:
            self.eng_sem[e] = self.es.enter_context(nc.semaphore("s_" + e))

    def sbuf(self, name, shape, dtype, G=1024):
        t = self.es.enter_context(self.nc.sbuf_tensor(name, list(shape), dtype))
        return T_(t[:], name, G)

    def psum(self, name, shape, dtype=F32, G=2048):
        t = self.es.enter_context(self.nc.psum_tensor(name, list(shape), dtype))
        return T_(t[:], name, G, excl=True)

    def _tag(self, tag):
        if tag not in self.tag_sem:
            self.tag_sem[tag] = self.es.enter_context(self.nc.semaphore("d_" + str(tag)))
            self.tag_cum[tag] = 0
        return self.tag_sem[tag]

    def op(self, eng, fn, reads=(), writes=(), dma_tag=None):
        idx = len(self.ops)
        rk = [k for v in reads if not v.excl for k in v.keys]
        wk = [k for v in writes for k in v.keys] + [k for v in reads if v.excl for k in v.keys]
        deps = set()
        for k in rk + wk:
            w = self.last_w.get(k)
            if w is not None:
                deps.add(w)
        for k in wk:
            r = self.readers.get(k)
            if r:
                deps.update(r.values())
        waits = []
        for d in deps:
            Dd = self.ops[d]
            if Dd["dma_tag"] is not None:
                waits.append(("dma", Dd["dma_tag"], self.tag_cum[Dd["dma_tag"]]))
            else:
                if Dd["eng"] == eng and eng == "pe":
                    continue
                Dd["flag"] = True
                waits.append(("eng", Dd["eng"], d))
        if dma_tag is not None:
            self._tag(dma_tag)
            self.tag_cum[dma_tag] += 16
        self.ops.append(dict(eng=eng, fn=fn, waits=waits, dma_tag=dma_tag, flag=False, count=None))
        wks = set(wk)
        for k in wks:
            self.last_w[k] = idx
            self.readers[k] = {}
        for k in rk:
            if k not in wks:
                rkey = eng if dma_tag is None else ("dma", idx)
                self.readers.setdefault(k, {})[rkey] = idx
        return idx

    def wait_all_dma(self, eng, tag):
        self.ops.append(dict(eng=eng, fn=None, waits=[("dma", tag, self.tag_cum[tag])], dma_tag=None,
                             flag=False, count=None))

    def dma(self, eng, out, in_, tag):
        return self.op(eng, lambda e: e.dma_start(out=out.ap, in_=in_.ap), reads=[in_], writes=[out], dma_tag=tag)

    def mm(self, out, lhsT, rhs, start=True, stop=True):
        return self.op("pe", lambda e: e.matmul(out.ap, lhsT.ap, rhs.ap, start=start, stop=stop),
                       reads=[lhsT, rhs], writes=[out])

    def transpose(self, out, in_, ident):
        return self.op("pe", lambda e: e.transpose(out.ap, in_.ap, ident.ap), reads=[in_, ident], writes=[out])

    def act(self, out, in_, func, bias=None, scale=None):
        reads = [in_]
        kw = {}
        if bias is not None:
            if isinstance(bias, V):
                reads.append(bias)
                kw["bias"] = bias.ap
            else:
                kw["bias"] = bias
        if scale is not None:
            if isinstance(scale, V):
                reads.append(scale)
                kw["scale"] = scale.ap
            else:
                kw["scale"] = scale
        return self.op("act", lambda e: e.activation(out.ap, in_.ap, func, **kw), reads=reads, writes=[out])

    def tt(self, out, a, b, op, eng="dve"):
        return self.op(eng, lambda e: e.tensor_tensor(out.ap, a.ap, b.ap, op), reads=[a, b], writes=[out])

    def ts(self, out, a, s1, op0, s2=None, op1=None, eng="dve"):
        reads = [a]
        x1 = s1
        if isinstance(s1, V):
            reads.append(s1)
            x1 = s1.ap
        x2 = s2
        if isinstance(s2, V):
            reads.append(s2)
            x2 = s2.ap
        if op1 is None:
            return self.op(eng, lambda e: e.tensor_scalar(out.ap, a.ap, x1, None, op0), reads=reads, writes=[out])
        return self.op(eng, lambda e: e.tensor_scalar(out.ap, a.ap, x1, x2, op0, op1), reads=reads, writes=[out])

    def stt(self, out, a, s, b, op0, op1, eng="dve"):
        reads = [a, b]
        x = s
        if isinstance(s, V):
            reads.append(s)
            x = s.ap
        return self.op(eng, lambda e: e.scalar_tensor_tensor(out.ap, a.ap, x, b.ap, op0, op1),
                       reads=reads, writes=[out])

    def copy(self, out, in_, eng="dve"):
        if eng == "act":
            return self.op(eng, lambda e: e.copy(out.ap, in_.ap), reads=[in_], writes=[out])
        return self.op(eng, lambda e: e.tensor_copy(out.ap, in_.ap), reads=[in_], writes=[out])

    def memset(self, out, val, eng="dve"):
        return self.op(eng, lambda e: e.memset(out.ap, val), reads=[], writes=[out])

    def reduce(self, out, in_, op, eng="dve"):
        return self.op(eng, lambda e: e.tensor_reduce(out.ap, in_.ap, AX.X, op), reads=[in_], writes=[out])

    def recip(self, out, in_):
        return self.op("dve", lambda e: e.reciprocal(out.ap, in_.ap), reads=[in_], writes=[out])

    def emit(self):
        nc = self.nc
        cnt = {e: 0 for e in self.ENGS}
        for o in self.ops:
            if o["dma_tag"] is None and o["flag"]:
                cnt[o["eng"]] += 1
                o["count"] = cnt[o["eng"]]
        engobj = {"pe": "tensor", "act": "scalar", "dve": "vector", "pool": "gpsimd", "sp": "sync"}
        stats = {e: [0, 0] for e in self.ENGS}
        with nc.Block() as block:
            for e in self.ENGS:
                mine = [o for o in self.ops if o["eng"] == e]

                def body(eng, mine=mine, e=e):
                    waited = {}
                    for o in mine:
                        need = {}
                        for w in o["waits"]:
                            if w[0] == "dma":
                                sem = self.tag_sem[w[1]]
                                val = w[2]
                                key = ("d", w[1])
                            else:
                                sem = self.eng_sem[w[1]]
                                val = self.ops[w[2]]["count"]
                                key = ("e", w[1])
                            if key not in need or need[key][1] < val:
                                need[key] = (sem, val)
                        for key, (sem, val) in need.items():
                            if waited.get(key, 0) < val:
                                eng.wait_ge(sem, val)
                                waited[key] = val
                                stats[e][1] += 1
                        if o["fn"] is None:
                            continue
                        ins = o["fn"](eng)
                        stats[e][0] += 1
                        if o["dma_tag"] is not None:
                            ins.then_inc(self.tag_sem[o["dma_tag"]], 16)
                        elif o["flag"]:
                            ins.then_inc(self.eng_sem[e], 1)

                getattr(block, engobj[e])(body)
        return stats

    def close(self):
        self.es.close()


class Ring:
    def __init__(self, items):
        self.items = items
        self.i = 0

    def next(self):
        x = self.items[self.i % len(self.items)]
        self.i += 1
        return x


class Arena:
    def __init__(self, P, name, nbytes, G=1024):
        self.t = P.es.enter_context(P.nc.sbuf_tensor(name, [128, nbytes // 2], BF16))
        self.name = name
        self.G = G
        self.nbytes = nbytes

    def view(self, off, shape, dtype, parts=128):
        n = 1
        for s in shape:
            n *= s
        nb = n * _dsize(dtype)
        assert off % 4 == 0 and off + nb <= self.nbytes, (off, nb, self.nbytes)
        ap = self.t[0:parts, off // 2:(off + nb) // 2]
        if dtype == F32:
            ap = ap.bitcast(F32)
        if len(shape) == 2:
            ap = ap.rearrange("p (a b) -> p a b", a=shape[0])
        elif len(shape) == 3:
            ap = ap.rearrange("p (a b c) -> p a b c", a=shape[0], b=shape[1])
        elif len(shape) == 4:
            ap = ap.rearrange("p (a b c d) -> p a b c d", a=shape[0], b=shape[1], c=shape[2])
        return T_(ap, self.name, self.G)


def build_program(nb=NB, nlayers=2, dbg_stage=None):
    nc = bass.Bass("TRN2", target_bir_lowering=False)
    P = Prog(nc)

    def din(name, shape, dt=F32):
        return nc.dram_tensor(name, list(shape), dt, kind="ExternalInput").ap()

    xT_d = din("xT", [nb, 128, 8, T])
    cT_d = din("cT", [128, 8, 5])
    wada_d = din("w_ada_r", [2, 48, 128, 8, 128])
    bada_d = din("b_adaT", [128, 2, 48])
    gnorm_d = din("g_normT", [128, 2, 2, 8])
    wfm_d = din("w_fm", [2, 17, 128, 8, 128])
    wtm_d = din("w_tm", [2, 128, 8, 768])
    qkg_d = din("qkg", [128, 2, 2])
    cos_d = din("cosT", [128, T])
    sin_d = din("sinT", [128, T])
    sink_d = din("sink", [128, 16])
    gw_d = din("gla_gw", [128, 2, 256])
    gng_d = din("gla_ng", [128, 2, 64])
    cw_d = din("conv_wT", [128, 2, 2, 3])
    wout_d = din("w_out_r", [2, 128, 8, 1024])
    wr_d = din("w_router_r", [128, 8, 16])
    br_d = din("b_router_r", [128, 16])
    wg_d = din("wg_r", [2, NEXP, 128, 8, 512])
    wu_d = din("wu_r", [2, NEXP, 128, 8, 512])
    wd_d = din("wd_r", [2, NEXP, 128, 4, 1024])
    cst_d = din("consts", [128, 12, 128])
    bdm_d = din("bdmask", [128, 256])
    sel_d = din("sel", [128, 16, 128])
    out_d = nc.dram_tensor("outT", [nb, 128, 8, S], F32, kind="ExternalOutput").ap()

    XT = P.sbuf("XT", [128, 8, T], F32, G=1024)
    HT = P.sbuf("HT", [128, 8, T], BF16, G=512)
    U = Arena(P, "U", 65536, G=256)
    U2 = Arena(P, "U2", 6144, G=256)
    IDENT = P.sbuf("IDENT", [128, 128], F32)
    CB16 = P.sbuf("CB16", [128, 8, 128], BF16, G=256)
    (C_ONES, C_LINC, C_USTR, C_UINC, C_LSTR, C_PERM, C_BD64, C_IDB) = range(8)
    HMASK = P.sbuf("HMASK", [128, 8], F32)
    BDM = P.sbuf("BDM", [128, 256], F32)
    MOD = P.sbuf("MOD", [128, 2, 48, 5], F32, G=64)
    GS = P.sbuf("GS", [128, 2, 2, 8, 5], F32, G=64)
    GN = P.sbuf("GN", [128, 2, 2, 8], F32, G=64)
    BADA = P.sbuf("BADA", [128, 2, 48], F32, G=64)
    CT = P.sbuf("CT", [128, 8, 5], F32)
    QKG = P.sbuf("QKG", [128, 2, 2], F32)
    CW = P.sbuf("CW", [128, 2, 2, 3], F32)
    GNG = P.sbuf("GNG", [128, 2, 64], F32)
    WR32 = P.sbuf("WR32", [128, 8, 16], F32)
    BRT = P.sbuf("BRT", [128, 16], F32, G=64)
    SKE = P.sbuf("SKE", [128, 16], F32, G=64)
    GWB = P.sbuf("GWB", [128, 2, 256], BF16)
    GP = P.sbuf("GP", [128, 4, 128], F32, G=2048)
    SQ = [P.sbuf("SQ%d" % i, [128, 512], BF16) for i in range(2)]
    RS = P.sbuf("RS", [128, 512], F32, G=2048)
    TMPF = [P.sbuf("TMPF%d" % i, [128, 512], F32, G=2048) for i in range(2)]
    HF = [U.view(45568 + i * 2048, [512], F32) for i in range(2)]
    SMALL = P.sbuf("SMALL", [128, 16, 64], F32, G=256)
    PS = [P.psum("PS%d" % i, [128, 512], F32) for i in range(7)]
    PT = P.psum("PT", [128, 1024], BF16)

    sqr = Ring(SQ)
    tmpr = Ring(TMPF)
    hfr = Ring(HF)

    def cst(i):
        return CB16[:, i, :]

    P.dma("sp", IDENT.all(), DR(cst_d[:, 11, :]), "c0")
    P.dma("pool", CB16[:, 0:8, :], DR(cst_d[:, 0:8, :]), "c1")
    P.dma("sp", HMASK.all(), DR(cst_d[:, 8, 0:8]), "c0")
    P.dma("sp", BDM.all(), DR(bdm_d), "c0")
    P.dma("sp", BADA.all(), DR(bada_d), "c0")
    P.dma("sp", GN.all(), DR(gnorm_d), "c0")
    P.dma("sp", CT.all(), DR(cT_d), "c0")
    P.dma("sp", QKG.all(), DR(qkg_d), "c0")
    P.dma("sp", CW.all(), DR(cw_d), "c0")
    P.dma("sp", GNG.all(), DR(gng_d), "c0")
    P.dma("sp", WR32.all(), DR(wr_d), "c0")
    P.dma("sp", BRT.all(), DR(br_d), "c0")
    P.dma("sp", SKE.all(), DR(sink_d), "c0")
    P.dma("pool", GWB.all(), DR(gw_d), "c1")
    P.memset(GP.all(), 0.0)
    P.act(SKE.all(), SKE.all(), AF.Exp)

    SC = P.sbuf("SC", [128, 8, 5], F32)
    P.act(SC.all(), CT.all(), AF.Exp, scale=-1.0)
    P.ts(SC.all(), SC.all(), 1.0, ALU.add)
    P.recip(SC.all(), SC.all())
    P.tt(SC.all(), SC.all(), CT.all(), ALU.mult)
    WA = [U.view(i * 4096, [8, 128], F32) for i in range(3)]
    war = Ring(WA)
    wa_tags = Ring(["wa0", "wa1", "wa2"])
    for l in range(nlayers):
        slots = {}

        def ld(n):
            w = war.next()
            P.dma("sp", w.all(), DR(wada_d[l, n]), wa_tags.next())
            slots[n] = w

        ld(0)
        ld(1)
        for n in range(48):
            if n + 2 < 48:
                ld(n + 2)
            w = slots.pop(n)
            for k in range(8):
                P.mm(PS[0][:, 0:5], w[:, k, :], SC[:, k, :], start=(k == 0), stop=(k == 7))
            P.act(MOD[:, l, n, :], PS[0][:, 0:5], AF.Identity, bias=BADA[:, l, n:n + 1], scale=1.0)
        for s in range(2):
            for c in range(8):
                P.ts(GS[:, l, s, c, :], MOD[:, l, (3 * s + 1) * 8 + c, :], 1.0, ALU.add,
                     GN[:, l, s, c:c + 1], ALU.mult)

    RBANK = [PS[1], PS[4], PS[5], PS[6]]

    def modv(l, kind, c, row):
        return MOD[:, l, kind * 8 + c, row:row + 1]

    def norm_phase(l, s, j, blks, router=None):
        for bi in blks:
            t0, t1 = BLKS[bi]
            n = t1 - t0
            row = 4 if bi == 0 else j
            for c in range(8):
                q = sqr.next()
                P.act(q[:, 0:n], XT[:, c, t0:t1], AF.Square)
                P.mm(PS[0][:, 0:n], cst(C_ONES), q[:, 0:n], start=(c == 0), stop=(c == 7))
            P.act(RS[:, 0:n], PS[0][:, 0:n], AF.Ln, bias=EPS, scale=1.0 / D)
            P.act(RS[:, 0:n], RS[:, 0:n], AF.Exp, scale=-0.5)
            for c in range(8):
                tm = tmpr.next()
                P.stt(tm[:, 0:n], XT[:, c, t0:t1], GS[:, l, s, c, row:row + 1], RS[:, 0:n], ALU.mult, ALU.mult)
                P.act(HT[:, c, t0:t1], tm[:, 0:n], AF.Identity, bias=modv(l, 3 * s, c, row), scale=1.0)
                if router is not None:
                    hf = hfr.next()
                    P.ts(hf[:, 0:n], tm[:, 0:n], modv(l, 3 * s, c, row), ALU.add)
                    for q4 in range(n // 128):
                        P.mm(RBANK[q4][:, 0:16], hf[:, q4 * 128:(q4 + 1) * 128], WR32[:, c, :],
                             start=(c == 0), stop=(c == 7))
            if router is not None:
                router(bi, n // 128)

    wfm_slots = [U.view(57344 + i * 2048, [8, 128], BF16) for i in range(2)]
    wfm_ring = Ring(wfm_slots)
    wfm_tags = Ring(["wf0", "wf1"])

    def load_wfm(l, ch):
        w = wfm_ring.next()
        P.dma("pool", w.all(), DR(wfm_d[l, ch]), wfm_tags.next())
        return w

    projps = Ring([PS[2], PS[3]])

    def proj_fm(w, bi):
        t0, t1 = BLKS[bi]
        ps = projps.next()
        for k in range(8):
            P.mm(ps[:, 0:t1 - t0], w[:, k, :], HT[:, k, t0:t1], start=(k == 0), stop=(k == 7))
        return ps

    WO = U.view(53248, [2, 1024], BF16)

    def wout_partial(l, j, kc0, YT, blks):
        P.dma("pool", WO.all(), DR(wout_d[l, :, kc0:kc0 + 2, :]), "wo")
        for bi in blks:
            t0, t1 = BLKS[bi]
            n = t1 - t0
            row = 4 if bi == 0 else j
            for d in range(8):
                ps = projps.next()
                for k in range(2):
                    P.mm(ps[:, 0:n], WO[:, k, d * 128:(d + 1) * 128], YT[:, k, t0:t1], start=(k == 0), stop=(k == 1))
                P.stt(XT[:, d, t0:t1], ps[:, 0:n], modv(l, 2, d, row), XT[:, d, t0:t1], ALU.mult, ALU.add)

    def conv_group(l, j, blks_out):
        CBf = U.view(0, [T], F32)
        CCf = U.view(9216, [T], F32)
        UU = U.view(18432, [T], F32)
        ACC = U.view(27648, [T], F32)
        YT = U.view(36864, [2, T], BF16)
        for cc in range(2):
            for which, dst in ((9 + cc, CBf), (11 + cc, CCf), (13 + cc, None)):
                w = load_wfm(l, which)
                for bi in range(5):
                    t0, t1 = BLKS[bi]
                    ps = proj_fm(w, bi)
                    if dst is not None:
                        P.copy(dst[:, t0:t1], ps[:, 0:t1 - t0], eng="act")
                    else:
                        P.tt(UU[:, t0:t1], ps[:, 0:t1 - t0], CCf[:, t0:t1], ALU.mult)
            P.ts(ACC.all(), UU.all(), CW[:, l, cc, 1:2], ALU.mult)
            for (a, b) in ((0, L), (L, T)):
                P.stt(ACC[:, a + 1:b], UU[:, a:b - 1], CW[:, l, cc, 0:1], ACC[:, a + 1:b], ALU.mult, ALU.add)
                P.stt(ACC[:, a:b - 1], UU[:, a + 1:b], CW[:, l, cc, 2:3], ACC[:, a:b - 1], ALU.mult, ALU.add)
            P.tt(YT[:, cc, :], CBf.all(), ACC.all(), ALU.mult)
        wout_partial(l, j, 6, YT, blks_out)

    def attn_group(l, j, kv, with_ctx_out):
        QT = U.view(0, [2, T], BF16)
        KM = U.view(9216, [2, T], BF16)
        VX = U.view(18432, [NT, 128], BF16)
        YT = U.view(23040, [2, T], BF16)
        COS = U.view(32256, [T], BF16)
        SIN = U.view(36864, [T], BF16)
        KN = U.view(41472, [512], BF16)
        QN = U.view(42496, [512], BF16)
        SQB = U.view(43520, [512], BF16)
        EX = [U.view(44544 + i * 1024, [512], BF16) for i in range(3)]
        RD = U.view(47616, [512], F32)
        WV = U.view(49664, [8, 64], BF16)
        RQ = U.view(50688, [512], F32)
        ESK = U.view(61440, [512], F32)
        DEN = U.view(63488, [512], F32)
        exr = Ring(EX)
        sqbr = Ring([SQB, U2.view(0, [512], BF16)])
        qnr = Ring([QN, U2.view(1024, [512], BF16)])
        knr = Ring([KN, U2.view(2048, [512], BF16)])
        rqr = Ring([RQ, U2.view(3072, [512], F32)])
        denr = Ring([DEN, U2.view(0, [512], F32)])
        rdr = Ring([RD, U2.view(2048, [512], F32)])
        ssps = Ring([PS[4], PS[0]])
        rotps = Ring([PS[5], PS[1]])
        P.dma("pool", COS.all(), DR(cos_d), "tab")
        P.dma("pool", SIN.all(), DR(sin_d), "tab")
        for a in range(2):
            for b in range(2):
                idx = 8 * l + 4 * kv + 2 * b + a
                P.copy(ESK[:, (2 * a + b) * 128:(2 * a + b + 1) * 128], SKE[:, idx:idx + 1].bcast([128, 128]), eng="act")
        VT = U.view(23040, [T], BF16)
        items = [(which, bi) for which in (2 * kv, 2 * kv + 1, 4 + kv, 15 + kv) for bi in range(5)]
        pring = Ring([PS[2], PS[3], PS[6]])
        wcur = {}
        stt_ = {}

        def s1(it):
            which, bi = it
            if which not in wcur:
                wcur.clear()
                wcur[which] = load_wfm(l, which)
            w = wcur[which]
            t0, t1 = BLKS[bi]
            n = t1 - t0
            ps = pring.next()
            for k in range(8):
                P.mm(ps[:, 0:n], w[:, k, :], HT[:, k, t0:t1], start=(k == 0), stop=(k == 7))
            if which >= 15:
                P.copy(VT[:, t0:t1], ps[:, 0:n], eng="act")
                return
            sqb = sqbr.next()
            P.act(sqb[:, 0:n], ps[:, 0:n], AF.Square)
            stt_[it] = [ps, sqb]

        def s2(it):
            which, bi = it
            if which >= 15:
                return
            isk = which >= 4
            t0, t1 = BLKS[bi]
            n = t1 - t0
            ps, sqb = stt_[it]
            ssp = ssps.next()
            rq = rqr.next()
            qn = qnr.next()
            P.mm(ssp[:, 0:n], cst(C_BD64), sqb[:, 0:n])
            P.act(rq[:, 0:n], ssp[:, 0:n], AF.Ln, bias=EPS, scale=1.0 / 64)
            P.act(rq[:, 0:n], rq[:, 0:n], AF.Exp, scale=-0.5)
            P.stt(qn[:, 0:n], ps[:, 0:n], QKG[:, l, (1 if isk else 0):(2 if isk else 1)], rq[:, 0:n],
                  ALU.mult, ALU.mult)
            stt_[it] = qn

        def s3(it):
            which, bi = it
            if which >= 15:
                return
            isk = which >= 4
            t0, t1 = BLKS[bi]
            n = t1 - t0
            qn = stt_.pop(it)
            rtp = rotps.next()
            P.mm(rtp[:, 0:n], cst(C_PERM), qn[:, 0:n])
            t1_ = tmpr.next()
            t2_ = tmpr.next()
            P.tt(t1_[:, 0:n], qn[:, 0:n], COS[:, t0:t1], ALU.mult)
            P.tt(t2_[:, 0:n], rtp[:, 0:n], SIN[:, t0:t1], ALU.mult)
            if not isk:
                P.tt(QT[:, which - 2 * kv, t0:t1], t1_[:, 0:n], t2_[:, 0:n], ALU.add)
            else:
                kn = knr.next()
                P.tt(kn[:, 0:n], t1_[:, 0:n], t2_[:, 0:n], ALU.add)
                P.ts(KM[:, 0, t0:t1], kn[:, 0:n], HMASK[:, 0:1], ALU.mult)
                P.ts(KM[:, 1, t0:t1], kn[:, 0:n], HMASK[:, 1:2], ALU.mult)

        for n_ in range(len(items) + 2):
            if n_ < len(items):
                s1(items[n_])
            if 0 <= n_ - 1 < len(items):
                s2(items[n_ - 1])
            if 0 <= n_ - 2 < len(items):
                s3(items[n_ - 2])
        for i0 in range(0, NT, 8):
            nt_ = min(8, NT - i0)
            for q in range(nt_):
                i = i0 + q
                P.transpose(PT[:, q * 128:(q + 1) * 128], VT[:, i * 128:(i + 1) * 128], cst(C_IDB))
            P.copy(VX[:, i0:i0 + nt_, :], PT[:, 0:nt_ * 128].re("p (q c) -> p q c", q=nt_), eng="act")
        stps = Ring([PS[0], PS[1]])
        numr = Ring([PS[2], PS[3]])
        dnr = Ring([PS[4], PS[5]])
        qtiles = list(range(2, NT)) + ([0, 1] if with_ctx_out else [])
        its = []
        for i in qtiles:
            if i >= 2:
                keys = [(0, None), (1, None)]
                if i - 1 >= 2:
                    keys.append((i - 1, C_UINC))
                keys.append((i, None))
                if i + 1 < NT:
                    keys.append((i + 1, C_LINC))
            else:
                keys = [(0, None), (1, None)]
            for ki, (kt, msk) in enumerate(keys):
                its.append((i, kt, msk, ki == 0, ki == len(keys) - 1))

        def scores(it):
            i, kt, msk, first, lastk = it
            st = stps.next()
            for par in range(2):
                P.mm(st[:, par * 256:(par + 1) * 256].re("p (c q) -> p c q", c=2),
                     KM[:, par, kt * 128:(kt + 1) * 128], QT[:, :, i * 128:(i + 1) * 128])
            return st

        cur = {}
        st_next = scores(its[0])
        for n_, it in enumerate(its):
            i, kt, msk, first, lastk = it
            st = st_next
            if n_ + 1 < len(its):
                st_next = scores(its[n_ + 1])
            if first:
                cur["num"] = numr.next()
                cur["den"] = dnr.next()
            ex = exr.next()
            P.act(ex.all(), st.all(), AF.Exp, scale=0.125)
            if msk is not None:
                P.tt(ex.all().re("p (h q) -> p h q", h=4), ex.all().re("p (h q) -> p h q", h=4),
                     cst(msk).bc(1, [128, 4, 128]), ALU.mult)
            P.mm(cur["num"].all(), VX[:, kt, :], ex.all(), start=first, stop=lastk)
            P.mm(cur["den"].all(), cst(C_ONES), ex.all(), start=first, stop=lastk)
            if lastk:
                den = denr.next()
                rd = rdr.next()
                P.tt(den.all(), cur["den"].all(), ESK.all(), ALU.add)
                P.act(rd.all(), den.all(), AF.Ln)
                P.act(rd.all(), rd.all(), AF.Exp, scale=-1.0)
                for par in range(2):
                    for cc in range(2):
                        cb = (2 * par + cc) * 128
                        P.tt(YT[par * 64:(par + 1) * 64, cc, i * 128:(i + 1) * 128],
                             cur["num"][par * 64:(par + 1) * 64, cb:cb + 128],
                             rd[par * 64:(par + 1) * 64, cb:cb + 128], ALU.mult)
        wout_partial(l, j, 2 * kv, YT, [0, 1, 2, 3, 4] if with_ctx_out else [1, 2, 3, 4])

    def gla_group(l, j, with_ctx_out):
        GQ = U.view(0, [T], BF16)
        GK = U.view(4608, [T], BF16)
        GLW = U.view(9216, [T], BF16)
        KTOK = U.view(13824, [NT, 128], BF16)
        VTOK = U.view(18432, [NT, 256], BF16)
        OF = U.view(27648, [NT, 256], BF16)
        YT = U.view(36864, [2, T], BF16)
        WRG = U.view(46080, [8, 256], BF16)
        WKV = U.view(50176, [8, 384], BF16)
        GALL = U.view(50176, [NT, 256], BF16)
        yb = U2.view(5632, [256], BF16)
        EB, ENB, ESUF = gla_f32[0], gla_f32[1], gla_f32[2]
        SETS = {
            (0, 0): dict(QB=U.view(61440, [128], BF16), KD=U.view(61696, [128], BF16), KB4=U.view(61952, [4, 128], BF16)),
            (0, 1): dict(QB=U2.view(0, [128], BF16), KD=U2.view(256, [128], BF16), KB4=U2.view(512, [4, 128], BF16)),
            (1, 0): dict(QB=U2.view(1536, [128], BF16), KD=U2.view(1792, [128], BF16), KB4=U2.view(2048, [4, 128], BF16)),
            (1, 1): dict(QB=U2.view(3072, [128], BF16), KD=U2.view(3328, [128], BF16), KB4=U2.view(3584, [4, 128], BF16)),
        }
        ATMS = [U.view(62976, [4, 128], BF16), U2.view(4608, [4, 128], BF16)]
        SBF = [U.view(64256 + d * 512, [256], BF16) for d in range(2)]
        S32 = [gla_s32[0], gla_s32[1]]
        KVMS = [gla_s32[2], gla_s32[3]]
        ATT_PS = [PS[0], PS[1]]
        bps = Ring([PS[4], PS[5]])

        P.dma("pool", WKV.all(), DR(wtm_d[l, :, :, 128:512]), "wkv")
        P.dma("pool", WRG.all(), DR(wtm_d[l, :, :, 512:768]), "wrg")
        for which, dst in ((6, GQ), (7, GK), (8, GLW)):
            w = load_wfm(l, which)
            for bi in range(5):
                t0, t1 = BLKS[bi]
                ps = proj_fm(w, bi)
                if which == 6:
                    P.act(dst[:, t0:t1], ps[:, 0:t1 - t0], AF.Identity, scale=32.0 ** -0.5, bias=0.0)
                else:
                    P.copy(dst[:, t0:t1], ps[:, 0:t1 - t0], eng="act")
        P.ts(GLW.all(), GLW.all(), HMASK[:, 6:7], ALU.add)
        for i0 in range(0, NT, 8):
            nt_ = min(8, NT - i0)
            for q in range(nt_):
                i = i0 + q
                P.transpose(PT[:, q * 128:(q + 1) * 128], GK[:, i * 128:(i + 1) * 128], cst(C_IDB))
            P.copy(KTOK[:, i0:i0 + nt_, :], PT[:, 0:nt_ * 128].re("p (q c) -> p q c", q=nt_), eng="act")
        kvr = Ring([PS[0], PS[1], PS[2], PS[3]])
        for i in range(NT):
            pv = kvr.next()
            for k in range(8):
                P.mm(pv[:, 0:256], HT[:, k, i * 128:(i + 1) * 128], WKV[:, k, 128:384], start=(k == 0), stop=(k == 7))
            P.copy(VTOK[:, i, :], pv[:, 0:256], eng="dve")
        zr = Ring([PS[5], PS[4]])
        for i in range(NT):
            zp = zr.next()
            P.mm(zp[:, 0:256], GLW[:, i * 128:(i + 1) * 128], GWB[:, l, :])
            e1 = tmpr.next()
            P.act(e1[:, 0:256], zp[:, 0:256], AF.Exp, scale=-1.0)
            P.act(e1[:, 0:256], e1[:, 0:256], AF.Ln, bias=1.0, scale=1.0)
            P.ts(GALL[:, i, :], e1[:, 0:256], -1.0 / 16.0, ALU.mult)

        order = [list(range(NT)), [1, 0] + list(range(NT - 1, 1, -1))]
        stepof = [{i: n for n, i in enumerate(order[d])} for d in range(2)]

        def stage_b(n, d):
            i = order[d][n]
            need_out = with_ctx_out or i >= 2
            X = SETS[(n % 2, d)]
            Mcum, Msuf = (C_LINC, C_USTR) if d == 0 else (C_UINC, C_LSTR)
            tcol = 127 if d == 0 else 0
            sl = slice(i * 128, (i + 1) * 128)
            g = GALL[:, i, d * 128:(d + 1) * 128]
            bp = bps.next()
            P.mm(bp[:, 0:128], g, cst(Mcum))
            P.mm(bp[:, 128:256], cst(Msuf), g)
            P.act(EB.all(), bp[:, 0:128], AF.Exp)
            P.act(ESUF.all(), bp[:, 128:256], AF.Exp)
            if need_out:
                P.act(ENB.all(), bp[:, 0:128], AF.Exp, scale=-1.0)
            slot = 2 * (n % 2) + d
            P.copy(SMALL[:, 15, slot:slot + 1], EB[:, tcol:tcol + 1], eng="act")
            P.tt(X["KD"].all(), KTOK[:, i, :], ESUF.all(), ALU.mult)
            if need_out:
                P.tt(X["QB"].all(), GQ[:, sl], EB.all(), ALU.mult)
                P.tt(ENB.all(), GK[:, sl], ENB.all(), ALU.mult)
                for h in range(4):
                    P.act(X["KB4"][:, h, :], ENB.all(), AF.Identity, scale=HMASK[:, 2 + h:3 + h], bias=0.0)

        def stage_c(n):
            tiles = [order[d][n] for d in range(2)]
            needs = [with_ctx_out or tiles[d] >= 2 for d in range(2)]
            finals = [stepof[1 - d][tiles[d]] < n for d in range(2)]
            Xs = [SETS[(n % 2, d)] for d in range(2)]
            for d in range(2):
                if needs[d]:
                    for h in range(4):
                        P.mm(ATT_PS[d][:, h * 128:(h + 1) * 128], Xs[d]["KB4"][:, h, :], Xs[d]["QB"].all())
            for d in range(2):
                P.mm(PS[2][:, 0:256] if d == 0 else PS[3][:, 0:256], Xs[d]["KD"].all(), VTOK[:, tiles[d], :])
            for d in range(2):
                if needs[d]:
                    Mmask = C_LINC if d == 0 else C_USTR
                    P.tt(ATMS[d].all(), ATT_PS[d].all().re("p (h q) -> p h q", h=4),
                         cst(Mmask).bc(1, [128, 4, 128]), ALU.mult)
            for d in range(2):
                slot = 2 * (n % 2) + d
                kvp = PS[2] if d == 0 else PS[3]
                P.tt(KVMS[d].all(), kvp[:, 0:256], BDM.all(), ALU.mult)
                P.stt(S32[d].all(), S32[d].all(), SMALL[:, 15, slot:slot + 1], KVMS[d].all(), ALU.mult, ALU.add)
            for d in range(2):
                if not needs[d]:
                    P.copy(SBF[d].all(), S32[d].all(), eng="act")
                    continue
                i = tiles[d]
                sl = slice(i * 128, (i + 1) * 128)
                for h in range(4):
                    hs = slice(h * 64, (h + 1) * 64)
                    P.mm(PS[6][:, hs], Xs[d]["QB"].all(), SBF[d][:, hs], start=True, stop=False)
                    P.mm(PS[6][:, hs], ATMS[d][:, h, :], VTOK[:, i, hs], start=False, stop=True)
                P.copy(SBF[d].all(), S32[d].all(), eng="act")
                if not finals[d]:
                    P.copy(OF[:, i, :], PS[6][:, 0:256], eng="act")
                    continue
                osum = tmpr.next()
                P.tt(osum[:, 0:256], PS[6][:, 0:256], OF[:, i, :], ALU.add)
                for k in range(8):
                    P.mm(PS[6][:, 256:512], HT[:, k, sl], WRG[:, k, :], start=(k == 0), stop=(k == 7))
                sq = tmpr.next()
                P.tt(sq[:, 0:256], osum[:, 0:256], osum[:, 0:256], ALU.mult)
                ssh = SMALL[:, 0, 0:4]
                P.reduce(ssh, sq[:, 0:256].re("p (h e) -> p h e", h=4), ALU.add)
                P.act(ssh, ssh, AF.Ln, bias=EPS, scale=1.0 / 64)
                P.act(ssh, ssh, AF.Exp, scale=-0.5)
                o3 = osum[:, 0:256].re("p (h e) -> p h e", h=4)
                P.tt(o3, o3, ssh.bc(2, [128, 4, 64]), ALU.mult)
                P.tt(o3, o3, GNG[:, l, :].bc(1, [128, 4, 64]), ALU.mult)
                er = sq
                P.act(er[:, 256:512], PS[6][:, 256:512], AF.Exp, scale=-1.0)
                P.act(er[:, 256:512], er[:, 256:512], AF.Ln, bias=1.0, scale=1.0)
                P.act(er[:, 256:512], er[:, 256:512], AF.Exp, scale=-1.0)
                P.tt(er[:, 256:512], er[:, 256:512], PS[6][:, 256:512], ALU.mult)
                P.tt(yb.all(), osum[:, 0:256], er[:, 256:512], ALU.mult)
                for c2 in range(2):
                    P.transpose(PT[:, c2 * 128:(c2 + 1) * 128], yb[:, c2 * 128:(c2 + 1) * 128], cst(C_IDB))
                P.copy(YT[:, :, sl], PT[:, 0:256].re("p (c q) -> p c q", c=2), eng="act")

        for d in range(2):
            P.memset(S32[d].all(), 0.0)
            P.memset(SBF[d].all(), 0.0)
        stage_b(0, 0)
        stage_b(0, 1)
        for n in range(NT):
            if n + 1 < NT:
                stage_b(n + 1, 0)
                stage_b(n + 1, 1)
            stage_c(n)
        wout_partial(l, j, 4, YT, [0, 1, 2, 3, 4] if with_ctx_out else [1, 2, 3, 4])

    gla_f32 = [P.sbuf("GF%d" % i, [128, 128], F32, G=512) for i in range(4)]
    gla_s32 = [P.sbuf("GS32_%d" % i, [128, 256], F32, G=1024) for i in range(4)]

    def moe_phase(l, j, blks):
        GT = U.view(40960, [T], BF16)
        ABUF = [U.view(45568 + i * 4096, [4, 512], BF16) for i in range(2)]
        SG = [U.view(53760 + i * 1024, [512], BF16) for i in range(2)]
        T1 = [U.view(55808 + i * 1024, [512], BF16) for i in range(2)]
        GBC = [U.view(57856 + i * 1024, [512], BF16) for i in range(2)]
        WSL = [U.view(i * 8192, [8, 512], BF16) for i in range(5)]
        WSLD = [U.view(i * 8192, [4, 1024], BF16) for i in range(5)]
        SEL = U.view(59904, [16, 128], BF16)
        P.dma("pool", SEL.all(), DR(sel_d), "sel")
        wring = Ring(list(range(5)))
        wtags = ["we%d" % i for i in range(5)]
        LG = SMALL

        def router(bi, nt):
            t0, _ = BLKS[bi]
            lgb = SMALL[:, 14, 0:nt * 16]
            lg3 = lgb.re("p (q e) -> p q e", q=nt)
            for q4 in range(nt):
                P.tt(lgb[:, q4 * 16:(q4 + 1) * 16], RBANK[q4][:, 0:16], BRT.all(), ALU.add)
            mx = SMALL[:, 0, 0:nt]
            P.reduce(mx, lg3, ALU.max)
            lgs = SMALL[:, 1, 0:nt * 16]
            P.tt(lgs.re("p (q e) -> p q e", q=nt), lg3, mx.bc(2, [128, nt, 16]), ALU.subtract)
            ex = SMALL[:, 2, 0:nt * 16]
            P.act(ex, lgs, AF.Exp)
            ng = nt * 4
            ex4 = ex.re("p (g f) -> p g f", g=ng)
            m1 = SMALL[:, 3, 0:ng]
            P.reduce(m1, ex4, ALU.max)
            eq = SMALL[:, 4, 0:nt * 16]
            eq4 = eq.re("p (g f) -> p g f", g=ng)
            P.tt(eq4, ex4, m1.bc(2, [128, ng, 4]), ALU.is_equal)
            e2 = SMALL[:, 5, 0:nt * 16]
            P.stt(e2, eq, -1.0e30, ex, ALU.mult, ALU.add)
            m2 = SMALL[:, 6, 0:ng]
            P.reduce(m2, e2.re("p (g f) -> p g f", g=ng), ALU.max)
            gsc = SMALL[:, 7, 0:ng]
            P.tt(gsc, m1, m2, ALU.add)
            gm = SMALL[:, 8, 0:nt]
            P.reduce(gm, gsc.re("p (q g) -> p q g", q=nt), ALU.max)
            gsel = SMALL[:, 9, 0:ng]
            P.tt(gsel.re("p (q g) -> p q g", q=nt), gsc.re("p (q g) -> p q g", q=nt), gm.bc(2, [128, nt, 4]),
                 ALU.is_equal)
            selx = SMALL[:, 10, 0:nt * 16]
            selx4 = selx.re("p (g f) -> p g f", g=ng)
            P.tt(selx4, ex4, m2.bc(2, [128, ng, 4]), ALU.is_ge)
            P.tt(selx4, selx4, gsel.bc(2, [128, ng, 4]), ALU.mult)
            wgt = SMALL[:, 11, 0:nt * 16]
            P.tt(wgt, ex, selx, ALU.mult)
            den = SMALL[:, 12, 0:nt]
            P.reduce(den, wgt.re("p (q e) -> p q e", q=nt), ALU.add)
            P.recip(den, den)
            P.tt(GP[:, 0:nt, 0:16], wgt.re("p (q e) -> p q e", q=nt), den.bc(2, [128, nt, 16]), ALU.mult)
            for q4 in range(nt):
                P.transpose(PS[6][:, q4 * 128:(q4 + 1) * 128], GP[:, q4, :], IDENT.all())
            P.copy(GT[:, t0:t0 + nt * 128], PS[6][:, 0:nt * 128], eng="act")

        norm_phase(l, 1, j, blks, router=router)
        if dbg_stage == "gates":
            P.copy(XT[:, 0, :], GT.all(), eng="act")
            return

        gps = Ring([PS[0], PS[1]])
        ups = Ring([PS[2], PS[3]])
        ops_ = Ring([PS[4], PS[5]])
        abr = Ring(ABUF)
        sgr = Ring(SG)
        t1r = Ring(T1)
        gbr = Ring(GBC)

        def load_gu(e):
            sl = [wring.next() for _ in range(3)]
            P.dma("pool", WSL[sl[0]].all(), DR(wg_d[l, e]), wtags[sl[0]])
            P.dma("pool", WSL[sl[1]].all(), DR(wu_d[l, e]), wtags[sl[1]])
            return sl

        def load_d(e, sl):
            P.dma("pool", WSLD[sl[2]].all(), DR(wd_d[l, e]), wtags[sl[2]])

        def gu_steps(e, bi, sl):
            t0, t1 = BLKS[bi]
            n = t1 - t0
            gb = gbr.next()
            ab = abr.next()

            def pre():
                P.mm(PS[6][:, 0:n], SEL[:, e, :], GT[:, t0:t1])
                P.copy(gb[:, 0:n], PS[6][:, 0:n], eng="act")

            def fstep(f):
                g = gps.next()
                u = ups.next()
                for k in range(8):
                    P.mm(g[:, 0:n], WSL[sl[0]][:, k, f * 128:(f + 1) * 128], HT[:, k, t0:t1], start=(k == 0), stop=(k == 7))
                for k in range(8):
                    P.mm(u[:, 0:n], WSL[sl[1]][:, k, f * 128:(f + 1) * 128], HT[:, k, t0:t1], start=(k == 0), stop=(k == 7))
                sg = sgr.next()
                P.act(sg[:, 0:n], g[:, 0:n], AF.Silu)
                t1_ = t1r.next()
                P.tt(t1_[:, 0:n], u[:, 0:n], sg[:, 0:n], ALU.mult)
                P.tt(ab[:, f, 0:n], t1_[:, 0:n], gb[:, 0:n], ALU.mult)

            return pre, fstep, ab

        def down_step(e, bi, sl, ab, d):
            t0, t1 = BLKS[bi]
            n = t1 - t0
            row = 4 if bi == 0 else j
            wd = WSLD[sl[2]]
            o = ops_.next()
            for f in range(4):
                P.mm(o[:, 0:n], wd[:, f, d * 128:(d + 1) * 128], ab[:, f, 0:n], start=(f == 0), stop=(f == 3))
            P.stt(XT[:, d, t0:t1], o[:, 0:n], modv(l, 5, d, row), XT[:, d, t0:t1], ALU.mult, ALU.add)

        pend = None
        nxt = load_gu(0)
        load_d(0, nxt)
        for e in range(NEXP):
            sl = nxt
            for bi in blks:
                pre, fstep, ab = gu_steps(e, bi, sl)
                pre()
                for f in range(4):
                    fstep(f)
                    if pend is not None:
                        down_step(*pend, 2 * f)
                        down_step(*pend, 2 * f + 1)
                pend = (e, bi, sl, ab)
                if bi == blks[0] and e + 1 < NEXP:
                    nxt = load_gu(e + 1)
            if e + 1 < NEXP:
                load_d(e + 1, nxt)
        for d in range(8):
            down_step(*pend, d)

    for j in range(nb):
        for c in range(8):
            P.dma("sp", XT[:, c, :], DR(xT_d[j, :, c, :]), "xin")
        for l in range(nlayers):
            last = (l == nlayers - 1) and nlayers == 2
            allb = [0, 1, 2, 3, 4]
            latb = [1, 2, 3, 4]
            norm_phase(l, 0, j, allb)
            if dbg_stage == "norm0":
                for c in range(8):
                    P.copy(XT[:, c, :], HT[:, c, :], eng="act")
                break
            conv_group(l, j, latb if last else allb)
            if dbg_stage == "conv":
                break
            gla_group(l, j, not last)
            if dbg_stage == "gla":
                break
            for kv in range(2):
                attn_group(l, j, kv, not last)
            if dbg_stage == "mix":
                break
            moe_phase(l, j, latb if last else allb)
            if dbg_stage == "gates":
                break
        for c in range(8):
            P.dma("sp", DR(out_d[j, :, c, :]), XT[:, c, L:T], "xout")
        if dbg_stage == "norm0":
            pass
    P.wait_all_dma("sp", "xout")
    stats = P.emit()
    P.close()
    return nc, stats


def _rope_tables():
    rows = S // 64
    r, c = np.meshgrid(np.arange(rows), np.arange(64), indexing="ij")
    nf = 16
    inv = (10000.0 ** (-np.arange(nf, dtype=np.float32) / nf)).astype(np.float32)
    ang = np.concatenate([r.reshape(-1, 1).astype(np.float32) * inv, c.reshape(-1, 1).astype(np.float32) * inv], -1)
    cos = np.cos(ang).astype(np.float32)
    sin = np.sin(ang).astype(np.float32)
    cosT = np.ones((128, T), np.float32)
    sinT = np.zeros((128, T), np.float32)
    for p in range(128):
        d = p % 64
        i = d % 32
        cosT[p, L:] = cos[:, i]
        sinT[p, L:] = (-sin[:, i]) if d < 32 else sin[:, i]
    return cosT, sinT


def _consts():
    c = np.zeros((128, 12, 128), np.float32)
    s = np.arange(128)[:, None]
    t = np.arange(128)[None, :]
    c[:, 0] = 1.0
    c[:, 1] = (s <= t)
    c[:, 2] = (s > t)
    c[:, 3] = (s >= t)
    c[:, 4] = (s < t)
    perm = np.zeros((128, 128), np.float32)
    for m in range(128):
        partner = m + 32 if (m % 64) < 32 else m - 32
        perm[partner, m] = 1.0
    c[:, 5] = perm
    c[:, 6] = ((s // 64) == (t // 64))
    c[:, 7] = np.eye(128)
    hm = np.zeros((128, 128), np.float32)
    hm[:64, 0] = 1.0
    hm[64:, 1] = 1.0
    for h in range(4):
        hm[32 * h:32 * h + 32, 2 + h] = 1.0
    hm[32, 6] = 1.0
    c[:, 8] = hm
    c[:, 11] = np.eye(128)
    bdm = ((np.arange(128)[:, None] // 32) == (np.arange(256)[None, :] // 64)).astype(np.float32)
    sel = np.zeros((128, 16, 128), np.float32)
    for e in range(16):
        sel[e, e, :] = 1.0
    return c, bdm, sel


def _prep_weights(inp):
    f = lambda a: np.ascontiguousarray(np.asarray(a, dtype=np.float32))
    w = {}
    w_ada = f(inp["w_ada"])
    w["w_ada_r"] = f(w_ada.reshape(2, 8, 128, 48, 128).transpose(0, 3, 2, 1, 4))
    w["b_adaT"] = f(f(inp["b_ada"]).reshape(2, 48, 128).transpose(2, 0, 1))
    gn = np.stack([f(inp["norm_mix_g"]), f(inp["norm_ffn_g"])], 1)
    w["g_normT"] = f(gn.reshape(2, 2, 8, 128).transpose(3, 0, 1, 2))
    w_in = f(inp["w_in"])
    cols = []
    for c in range(4):
        cols.append(np.arange(c * 128, (c + 1) * 128))
    cols.append(np.concatenate([np.arange(512, 576), np.arange(512, 576)]))
    cols.append(np.concatenate([np.arange(576, 640), np.arange(576, 640)]))
    cols.append(np.arange(768, 896))
    cols.append(np.arange(896, 1024))
    cols.append(None)
    for b0 in (1568, 1824, 2080):
        cols.append(np.arange(b0, b0 + 128))
        cols.append(np.arange(b0 + 128, b0 + 256))
    cols.append(np.concatenate([np.arange(640, 704), np.arange(640, 704)]))
    cols.append(np.concatenate([np.arange(704, 768), np.arange(704, 768)]))
    wfm = np.zeros((2, 17, 1024, 128), np.float32)
    for i, cidx in enumerate(cols):
        if cidx is None:
            wfm[:, i, :, 0:32] = w_in[:, :, 1536:1568]
        else:
            wfm[:, i] = w_in[:, :, cidx]
    w["w_fm"] = f(wfm.reshape(2, 17, 8, 128, 128).transpose(0, 1, 3, 2, 4))
    tmc = np.concatenate([np.arange(640, 768), np.arange(896, 1024), np.arange(1024, 1280), np.arange(1280, 1536)])
    w["w_tm"] = f(w_in[:, :, tmc].reshape(2, 8, 128, 768).transpose(0, 2, 1, 3))
    qg = f(inp["q_norm_g"])
    kg = f(inp["k_norm_g"])
    qkg = np.zeros((128, 2, 2), np.float32)
    for p in range(128):
        qkg[p, :, 0] = qg[:, p % 64]
        qkg[p, :, 1] = kg[:, p % 64]
    w["qkg"] = qkg
    w["cosT"], w["sinT"] = _rope_tables()
    w["sink"] = f(np.broadcast_to(f(inp["attn_sink"]).reshape(1, 16), (128, 16)))
    gw = f(inp["gla_gate_w"])
    gwm = np.zeros((128, 2, 256), np.float32)
    gwm[0:16, :, 0:128] = gw[:, 0].transpose(1, 0, 2)
    gwm[16:32, :, 128:256] = gw[:, 1].transpose(1, 0, 2)
    gwm[32] = f(inp["gla_gate_b"]).reshape(2, 256)
    w["gla_gw"] = gwm
    w["gla_ng"] = f(np.broadcast_to(f(inp["gla_norm_g"])[None], (128, 2, 64)))
    cw = f(inp["conv_w"])
    w["conv_wT"] = f(cw.reshape(2, 3, 2, 128).transpose(3, 0, 2, 1))
    w["w_out_r"] = f(f(inp["w_out"]).reshape(2, 8, 128, 1024).transpose(0, 2, 1, 3))
    w["w_router_r"] = f(f(inp["w_router"]).reshape(8, 128, 16).transpose(1, 0, 2))
    w["b_router_r"] = f(np.broadcast_to(f(inp["b_router"]).reshape(1, 16), (128, 16)))
    w["wg_r"] = f(f(inp["w_gate_e"]).reshape(2, 16, 8, 128, 512).transpose(0, 1, 3, 2, 4))
    w["wu_r"] = f(f(inp["w_up_e"]).reshape(2, 16, 8, 128, 512).transpose(0, 1, 3, 2, 4))
    w["wd_r"] = f(f(inp["w_down_e"]).reshape(2, 16, 4, 128, 1024).transpose(0, 1, 3, 2, 4))
    c, bdm, sel = _consts()
    w["consts"] = c
    w["bdmask"] = bdm
    w["sel"] = sel
    return w


def _prep_core(inp, b0, nb):
    x = np.asarray(inp["x"], dtype=np.float32)[b0:b0 + nb]
    ctx = np.asarray(inp["ctx"], dtype=np.float32)[b0:b0 + nb]
    full = np.concatenate([ctx, x], axis=1)
    xT = np.ascontiguousarray(full.reshape(nb, T, 8, 128).transpose(0, 3, 2, 1))
    c5 = np.zeros((5, D), np.float32)
    cc = np.asarray(inp["c"], dtype=np.float32)[b0:b0 + nb]
    c5[:nb] = cc
    c5[4] = np.asarray(inp["c_ctx"], dtype=np.float32)
    cT = np.ascontiguousarray(c5.reshape(5, 8, 128).transpose(2, 1, 0))
    return {"xT": xT, "cT": cT}


_CACHE = {}


def kernel(**inputs):
    w = _prep_weights(inputs)
    if "nc" not in _CACHE:
        _CACHE["nc"] = build_program(NB, 2)[0]
    nc = _CACHE["nc"]
    in_maps = []
    for core in range(NCORES):
        m = dict(w)
        m.update(_prep_core(inputs, core * NB, NB))
        in_maps.append(m)
    res = run_bass_kernel_spmd(nc, in_maps, core_ids=list(range(NCORES)))
    outs = []
    for core in range(NCORES):
        o = np.asarray(res.results[core]["outT"])
        outs.append(o.transpose(0, 3, 2, 1).reshape(NB, S, D))
    return np.ascontiguousarray(np.concatenate(outs, axis=0).astype(np.float32))
```
